# Optimizing a Trainium2 kernel written in Bass

```python
import math
import jax, jax.numpy as jnp
from jax import lax
import numpy as np

D_MODEL = 1024
BATCH = 4
SEQ = 4096
DEPTH = 1

D_MIX = D_MODEL
D_RWKV = D_MIX // 2
RWKV_HEAD = 64
RWKV_HEADS = D_RWKV // RWKV_HEAD
D_GMLP = D_MIX - D_RWKV
GMLP_GROUPS = 4
GMLP_GROUP_W = D_GMLP // GMLP_GROUPS
CHUNK = 128
DECAY_LORA = 64
ICLR_LORA = 64
GATE_LORA = 128
N_SHIFT = 3 * D_RWKV + DECAY_LORA + ICLR_LORA + GATE_LORA
D_IN = N_SHIFT + 2 * D_GMLP
D_PLE = 256
N_GROUPS = 4
EXPERTS_PER_GROUP = 8
N_EXPERTS = N_GROUPS * EXPERTS_PER_GROUP
TOP_K = 2
D_EXPERT = 512
EXPERT_BLOCK = 128
ALPHA = (2.0 * DEPTH) ** 0.25
BETA = (8.0 * DEPTH) ** -0.25
LN_EPS = 1e-5
GN_EPS = 64e-5
DECAY_SCALE = math.exp(-0.5)

kernel_name = "hymba_rwkv7_gmlp_hmoe_deepnorm"


def layer_norm(x, g, b, eps=LN_EPS):
    xf = x.astype(jnp.float32)
    mu = xf.mean(-1, keepdims=True)
    var = jnp.square(xf - mu).mean(-1, keepdims=True)
    return ((xf - mu) * lax.rsqrt(var + eps) * g + b).astype(x.dtype)


def token_shift(h):
    return jnp.pad(h, ((0, 0), (1, 0), (0, 0)))[:, :-1]


def wkv7_scan(r, w, k, v, a, b):
    Bn, Sn, H, N = r.shape

    def step(state, inp):
        r_t, w_t, k_t, v_t, a_t, b_t = inp
        sa = jnp.einsum('bhij,bhj->bhi', state, a_t)
        state = (state * w_t[:, :, None, :] + sa[..., None] * b_t[:, :, None, :]
                 + v_t[..., None] * k_t[:, :, None, :])
        y = jnp.einsum('bhij,bhj->bhi', state, r_t)
        return state, y

    xs = (jnp.moveaxis(t, 1, 0) for t in (r, w, k, v, a, b))
    s0 = jnp.zeros((Bn, H, N, N), jnp.float32)
    _, y = lax.scan(step, s0, tuple(xs))
    return jnp.moveaxis(y, 0, 1)


def rwkv7_mix(proj, mu, w0, w_decay_up, a0, w_iclr_up, w_gate_up, k_k, k_a, r_k, gn_g, gn_b):
    Bn, Sn, _ = proj.shape
    f32 = jnp.float32
    h = proj[..., :N_SHIFT]
    h = h + (token_shift(h) - h) * mu
    c1, c2, c3 = D_RWKV, 2 * D_RWKV, 3 * D_RWKV
    r, k, v, xw, xa, xg = jnp.split(h, [c1, c2, c3, c3 + DECAY_LORA, c3 + DECAY_LORA + ICLR_LORA], axis=-1)
    d = (w0 + jnp.tanh(xw) @ w_decay_up).astype(f32)
    w = jnp.exp(-DECAY_SCALE * jax.nn.sigmoid(d))
    a = jax.nn.sigmoid(a0 + xa @ w_iclr_up)
    g = jax.nn.sigmoid(xg) @ w_gate_up
    heads = lambda t: t.reshape(Bn, Sn, RWKV_HEADS, RWKV_HEAD).astype(f32)
    kk = heads(k * k_k)
    kk = kk / jnp.maximum(jnp.sqrt(jnp.sum(kk * kk, -1, keepdims=True)), 1e-12)
    k = k * (1 + (a - 1) * k_a)
    rh, kh, vh, ah, wh = heads(r), heads(k), heads(v), heads(a), heads(w)
    y = wkv7_scan(rh, wh, kh, vh, -kk, kk * ah)
    m = y.mean(-1, keepdims=True)
    var = jnp.square(y - m).mean(-1, keepdims=True)
    y = ((y - m) * lax.rsqrt(var + GN_EPS)).reshape(Bn, Sn, D_RWKV) * gn_g + gn_b
    bonus = jnp.sum(rh * kh * r_k, -1, keepdims=True) * vh
    y = y + bonus.reshape(Bn, Sn, D_RWKV)
    return (y * g).astype(proj.dtype)


def gmlp_mix(proj, ln_g, ln_b, w_spatial, b_spatial):
    Bn, Sn, _ = proj.shape
    n_chunks = Sn // CHUNK
    z = jax.nn.gelu(proj[..., N_SHIFT:], approximate=False)
    zu, zv = jnp.split(z, 2, axis=-1)
    shp = (Bn, n_chunks, CHUNK, GMLP_GROUPS, GMLP_GROUP_W)
    zv = layer_norm(zv.reshape(shp), ln_g.reshape(GMLP_GROUPS, GMLP_GROUP_W),
                    ln_b.reshape(GMLP_GROUPS, GMLP_GROUP_W))
    causal = jnp.tril(jnp.ones((CHUNK, CHUNK), dtype=bool))
    ws = jnp.where(causal, w_spatial, 0)
    mixed = jnp.einsum('gts,bcsgd->bctgd', ws, zv) + b_spatial.T[:, :, None]
    return (zu.reshape(shp) * mixed).reshape(Bn, Sn, D_GMLP)


def hmoe(x, w_group_router, b_group_router, w_expert_router, b_expert_router, w_gate, w_up, w_down):
    Bn, Sn, D = x.shape
    f32 = jnp.float32
    T = Bn * Sn
    xf = x.reshape(T, D)
    g_logits = (xf @ w_group_router).astype(f32) + b_group_router
    g_prob = jax.nn.softmax(g_logits, -1)
    g_sel = jnp.argmax(g_logits, -1).astype(jnp.int32)
    p_group = jnp.take_along_axis(g_prob, g_sel[:, None], -1)
    e_all = (xf @ w_expert_router.reshape(D, N_GROUPS * EXPERTS_PER_GROUP)).astype(f32)
    e_all = e_all.reshape(T, N_GROUPS, EXPERTS_PER_GROUP) + b_expert_router
    e_logits = jnp.take_along_axis(e_all, g_sel[:, None, None], 1)[:, 0]
    top_v, top_i = lax.top_k(e_logits, TOP_K)
    gate_w = jax.nn.softmax(top_v, -1) * p_group
    expert_id = g_sel[:, None] * EXPERTS_PER_GROUP + top_i.astype(jnp.int32)
    A = T * TOP_K
    flat_e = expert_id.reshape(A)
    flat_w = gate_w.reshape(A)
    flat_tok = (jnp.arange(A, dtype=jnp.int32) // TOP_K)
    order = jnp.argsort(flat_e)
    e_sorted = flat_e[order]
    counts = jnp.zeros((N_EXPERTS,), jnp.int32).at[flat_e].add(1)
    padded = (counts + EXPERT_BLOCK - 1) // EXPERT_BLOCK * EXPERT_BLOCK
    pad_end = jnp.cumsum(padded)
    pad_start = pad_end - padded
    start = jnp.cumsum(counts) - counts
    dest = pad_start[e_sorted] + jnp.arange(A, dtype=jnp.int32) - start[e_sorted]
    n_blocks = -(-A // EXPERT_BLOCK) + N_EXPERTS
    M = n_blocks * EXPERT_BLOCK
    row_tok = jnp.full((M,), T, jnp.int32).at[dest].set(flat_tok[order])
    row_w = jnp.zeros((M,), f32).at[dest].set(flat_w[order])
    block_e = jnp.minimum(jnp.searchsorted(pad_end, jnp.arange(n_blocks) * EXPERT_BLOCK, side='right'),
                          N_EXPERTS - 1)
    x_pad = jnp.concatenate([xf, jnp.zeros((1, D), xf.dtype)], 0)
    xb = x_pad[row_tok].reshape(n_blocks, EXPERT_BLOCK, D)

    def expert_block(args):
        xblk, e = args
        hid = jax.nn.silu(xblk @ w_gate[e]) * (xblk @ w_up[e])
        return hid @ w_down[e]

    yb = lax.map(expert_block, (xb, block_e)).reshape(M, D)
    y = jnp.zeros((T + 1, D), f32).at[row_tok].add(yb.astype(f32) * row_w[:, None])
    return y[:T].reshape(Bn, Sn, D).astype(x.dtype)


def setup_inputs(seed: int = 0) -> dict:
    key = jax.random.key(seed)
    ks = iter(jax.random.split(key, 48))
    nrm = lambda shape, scale: jax.random.normal(next(ks), shape, jnp.float32) * scale
    L, D = DEPTH, D_MODEL
    return {
        "x": nrm((BATCH, SEQ, D), 1.0),
        "p": nrm((DEPTH, BATCH, SEQ, D_PLE), 1.0),
        "ln_emb_g": 1.0 + nrm((D,), 0.02),
        "ln_emb_b": nrm((D,), 0.02),
        "w_in": nrm((L, D, D_IN), D ** -0.5),
        "mu_shift": jax.random.uniform(next(ks), (L, N_SHIFT), jnp.float32),
        "w0": -2.0 + nrm((L, D_RWKV), 1.0),
        "w_decay_up": nrm((L, DECAY_LORA, D_RWKV), 0.1 * DECAY_LORA ** -0.5),
        "a0": nrm((L, D_RWKV), 0.5),
        "w_iclr_up": nrm((L, ICLR_LORA, D_RWKV), 0.5 * ICLR_LORA ** -0.5),
        "w_gate_up": nrm((L, GATE_LORA, D_RWKV), GATE_LORA ** -0.5),
        "k_k": 0.85 + nrm((L, D_RWKV), 0.02),
        "k_a": 1.0 + nrm((L, D_RWKV), 0.02),
        "r_k": nrm((L, RWKV_HEADS, RWKV_HEAD), 0.1),
        "gn_g": 1.0 + nrm((L, D_RWKV), 0.02),
        "gn_b": nrm((L, D_RWKV), 0.02),
        "gmlp_ln_g": 1.0 + nrm((L, D_GMLP), 0.02),
        "gmlp_ln_b": nrm((L, D_GMLP), 0.02),
        "w_spatial": nrm((L, GMLP_GROUPS, CHUNK, CHUNK), 0.02),
        "b_spatial": 1.0 + nrm((L, GMLP_GROUPS, CHUNK), 0.02),
        "w_out": nrm((L, D_MIX, D), BETA * D_MIX ** -0.5),
        "ln1_g": 1.0 + nrm((L, D), 0.02),
        "ln1_b": nrm((L, D), 0.02),
        "w_group_router": nrm((L, D, N_GROUPS), D ** -0.5),
        "b_group_router": nrm((L, N_GROUPS), 0.01),
        "w_expert_router": nrm((L, D, N_GROUPS, EXPERTS_PER_GROUP), D ** -0.5),
        "b_expert_router": nrm((L, N_GROUPS, EXPERTS_PER_GROUP), 0.01),
        "w_exp_gate": nrm((L, N_EXPERTS, D, D_EXPERT), D ** -0.5),
        "w_exp_up": nrm((L, N_EXPERTS, D, D_EXPERT), D ** -0.5),
        "w_exp_down": nrm((L, N_EXPERTS, D_EXPERT, D), BETA * D_EXPERT ** -0.5),
        "w_ple_gate": nrm((L, D, D), D ** -0.5),
        "b_ple_gate": nrm((L, D), 0.02),
        "w_ple_proj": nrm((L, D_PLE, D), BETA * D_PLE ** -0.5),
        "ln2_g": 1.0 + nrm((L, D), 0.02),
        "ln2_b": nrm((L, D), 0.02),
    }


def reference(x, p, ln_emb_g, ln_emb_b, w_in, mu_shift, w0, w_decay_up, a0, w_iclr_up, w_gate_up,
              k_k, k_a, r_k, gn_g, gn_b, gmlp_ln_g, gmlp_ln_b, w_spatial, b_spatial, w_out,
              ln1_g, ln1_b, w_group_router, b_group_router, w_expert_router, b_expert_router,
              w_exp_gate, w_exp_up, w_exp_down, w_ple_gate, b_ple_gate, w_ple_proj, ln2_g, ln2_b):
    x = layer_norm(x, ln_emb_g, ln_emb_b)
    for i in range(DEPTH):
        proj = x @ w_in[i]
        y_a = rwkv7_mix(proj, mu_shift[i], w0[i], w_decay_up[i], a0[i], w_iclr_up[i], w_gate_up[i],
                        k_k[i], k_a[i], r_k[i], gn_g[i], gn_b[i])
        y_b = gmlp_mix(proj, gmlp_ln_g[i], gmlp_ln_b[i], w_spatial[i], b_spatial[i])
        mix = jnp.concatenate([y_a, y_b], -1) @ w_out[i]
        x = layer_norm(ALPHA * x + mix, ln1_g[i], ln1_b[i])
        ffn = hmoe(x, w_group_router[i], b_group_router[i], w_expert_router[i], b_expert_router[i],
                   w_exp_gate[i], w_exp_up[i], w_exp_down[i])
        ple = jax.nn.sigmoid(x @ w_ple_gate[i] + b_ple_gate[i]) * (p[i] @ w_ple_proj[i])
        x = layer_norm(ALPHA * x + ffn + ple, ln2_g[i], ln2_b[i])
    return x
```

```python
import math
from contextlib import ExitStack
import numpy as np
import ml_dtypes
import concourse.bass as bass
import concourse.mybir as mybir
from concourse.bass_utils import run_bass_kernel_spmd

F32 = mybir.dt.float32
BF16 = mybir.dt.bfloat16
AF = mybir.ActivationFunctionType
ALU = mybir.AluOpType
AX = mybir.AxisListType

D = 1024
SEQ = 4096
NB = 4
HALF = 2048
NT_OWN = 16
NH = 8
DS = math.exp(-0.5)
ALPHA = 2.0 ** 0.25
LN_EPS = 1e-5
GN_EPS = 64e-5
NEXP = 32
DEXP = 512
PIPELINE = True
L2_DVE = False
DBG_STOP = None
SIM_MODE = False
N_TILES = 32
OWN_FROM = 16
CAP = 256
ROW = 1028
I32 = mybir.dt.int32


class Prog:
    CE = ("pe", "act", "dve", "pool")

    def __init__(self, nc, es, n_dma_sems=24):
        self.nc = nc
        self.q = {e: [] for e in ("pe", "act", "dve", "pool", "sp")}
        self.cnt = {e: 0 for e in self.CE}
        self.lastw = {}
        self.readers = {}
        self.csem = {e: es.enter_context(nc.semaphore("cs_" + e)) for e in self.CE}
        self.dsem = [es.enter_context(nc.semaphore("ds%d" % i)) for i in range(n_dma_sems)]
        self.dcnt = [0] * n_dma_sems
        self.n_rr = n_dma_sems
        self.es = es
        self.drr = 0
        self.all_dma = []
        self.bar = set()
        self.reg_req = {}
        self.regs = {}

    def barrier(self):
        b = set()
        for e in self.CE:
            if self.cnt[e] > 0:
                b.add(("c", e, self.cnt[e]))
        for i, c in enumerate(self.dcnt):
            if c > 0:
                b.add(("d", i, c))
        self.bar = b

    def _deps(self, reads, writes):
        deps = set()
        for b in reads:
            if b in self.lastw:
                deps.add(self.lastw[b])
        for b in writes:
            if b in self.lastw:
                deps.add(self.lastw[b])
            deps.update(self.readers.get(b, ()))
        deps |= self.bar
        return deps

    def _commit(self, tok, reads, writes):
        for b in reads:
            self.readers.setdefault(b, []).append(tok)
        for b in writes:
            self.lastw[b] = tok
            self.readers[b] = []

    def op(self, eng, fns, reads=(), writes=()):
        if not isinstance(fns, (list, tuple)):
            fns = [fns]
        deps = self._deps(reads, writes)
        self.cnt[eng] += 1
        tok = ("c", eng, self.cnt[eng])
        self.q[eng].append(("op", list(fns), deps, tok))
        self._commit(tok, reads, writes)
        return tok

    def dma(self, eng, fn, reads=(), writes=()):
        deps = self._deps(reads, writes)
        if eng == "pool" and SIM_MODE:
            self.dsem.append(self.es.enter_context(self.nc.semaphore("dp%d" % len(self.dsem))))
            self.dcnt.append(0)
            s = len(self.dsem) - 1
        else:
            s = self.drr
            self.drr = (self.drr + 1) % self.n_rr
        prev = self.dcnt[s]
        self.dcnt[s] += 16
        tok = ("d", s, self.dcnt[s])
        self.q[eng].append(("dma", fn, deps, tok, prev))
        self._commit(tok, reads, writes)
        self.all_dma.append(tok)
        return tok

    def replay(self, block, final_eng="sp"):
        prog = self

        def run(ename, e):
            waited = {}
            if ename == "pool":
                for nm, val in prog.reg_req.items():
                    prog.regs[nm] = e.to_reg(val)

            def wait(tok):
                if tok[0] == "c":
                    if tok[1] == ename and ename == "pe":
                        return
                    key = ("c", tok[1])
                    sem = prog.csem[tok[1]]
                else:
                    key = ("d", tok[1])
                    sem = prog.dsem[tok[1]]
                if waited.get(key, 0) >= tok[2]:
                    return
                waited[key] = tok[2]
                e.wait_ge(sem, tok[2])

            for item in prog.q[ename]:
                if item[0] == "op":
                    _, fns, deps, tok = item
                    for d in sorted(deps):
                        wait(d)
                    for f in fns[:-1]:
                        f(e)
                    fns[-1](e).then_inc(prog.csem[ename], 1)
                else:
                    _, fn, deps, tok, prev = item
                    for d in sorted(deps):
                        wait(d)
                    if prev > 0:
                        wait(("d", tok[1], prev))
                    fn(e).then_inc(prog.dsem[tok[1]], 16)
            if ename == final_eng:
                for s in range(len(prog.dsem)):
                    if prog.dcnt[s] > 0:
                        wait(("d", s, prog.dcnt[s]))
                for ce in prog.CE:
                    if prog.cnt[ce] > 0:
                        wait(("c", ce, prog.cnt[ce]))

        block.tensor(lambda e: run("pe", e))
        block.scalar(lambda e: run("act", e))
        block.vector(lambda e: run("dve", e))
        block.gpsimd(lambda e: run("pool", e))
        block.sync(lambda e: run("sp", e))


def MM(out, l, r, st=True, sp=True):
    return lambda e: e.matmul(out, l, r, start=st, stop=sp)


def TR(out, in_, ident):
    return lambda e: e.transpose(out, in_, ident)


def ACTF(out, in_, func, **kw):
    return lambda e: e.activation(out=out, in_=in_, func=func, **kw)


def TT(out, a, b, op):
    return lambda e: e.tensor_tensor(out=out, in0=a, in1=b, op=op)


def TS(out, a, s1, s2=None, op0=ALU.mult, op1=None):
    if op1 is None:
        return lambda e: e.tensor_scalar(out=out, in0=a, scalar1=s1, scalar2=None, op0=op0)
    return lambda e: e.tensor_scalar(out=out, in0=a, scalar1=s1, scalar2=s2, op0=op0, op1=op1)


def STT(out, in0, scalar, in1, op0, op1):
    return lambda e: e.scalar_tensor_tensor(out=out, in0=in0, scalar=scalar, in1=in1, op0=op0, op1=op1)


def CP(out, in_):
    return lambda e: e.tensor_copy(out, in_)


def RED(out, in_, op=ALU.add):
    return lambda e: e.tensor_reduce(out=out, in_=in_, axis=AX.X, op=op)


class Arena:
    def __init__(self, big, nbytes):
        self.big = big
        self.n = nbytes
        self.off = 0
        self.mark_ = 0

    def alloc(self, shape, dt):
        esz = 2 if dt == BF16 else 4
        n = 1
        for d in shape[1:]:
            n *= d
        nb = n * esz
        self.off = (self.off + 63) // 64 * 64
        o = self.off
        self.off += nb
        assert self.off <= self.n, ("SBUF arena overflow", self.off, self.n)
        v = self.big[0:shape[0], o // 2:(o + nb) // 2]
        if dt != BF16:
            v = v.bitcast(dt)
        if len(shape) == 3:
            v = v.rearrange("p (a b) -> p a b", a=shape[1])
        elif len(shape) == 4:
            v = v.rearrange("p (a b c) -> p a b c", a=shape[1], b=shape[2])
        return v


VEC1 = (("lneg", D), ("lneb", D), ("mu", 1792), ("w0", 512), ("a0", 512), ("kk", 512), ("ka", 512),
        ("rk", 512), ("gng", 512), ("gnb", 512), ("glg", 512), ("glb", 512))
VEC2 = (("l1g", D), ("l1b", D), ("l2g", D), ("l2b", D), ("bpg", D), ("brt", 36), ("eC", 32), ("pid", 1))


def _offs(spec):
    d = {}
    o = 0
    for nm, n in spec:
        d[nm] = (o, n)
        o += n
    return d, o


def build(debug=None):
    nc = bass.Bass("TRN2", target_bir_lowering=False)
    es = ExitStack()

    def din(name, shape, dt=F32):
        return nc.dram_tensor(name, list(shape), dt, kind="ExternalInput").ap()

    xs = din("xs", [SEQ, D])
    pin = din("p_s", [HALF, 256])
    flag_d = din("flag", [128, 1])
    w_in = din("w_in", [D, 2816])
    w_out = din("w_out", [D, D])
    wlora_d = din("wlora", [128, 512])
    wgate_d = din("wgate", [128, 512])
    wrt_d = din("wrouter", [D, 36])
    w_eg = din("w_exp_gate", [NEXP, D, DEXP])
    w_eu = din("w_exp_up", [NEXP, D, DEXP])
    w_ed = din("w_exp_down", [NEXP, DEXP, D])
    w_pg = din("w_ple_gate", [D, D])
    w_pp = din("w_ple_proj", [256, D])
    wsT_d = din("wsT", [128, 512])
    bsp_d = din("bsp", [128, 4])
    O1, NV1 = _offs(VEC1)
    O2, NV2 = _offs(VEC2)
    vec1_d = din("vec1", [128, NV1])
    vec2_d = din("vec2", [128, NV2])
    cm_d = din("cmats", [128, 6 * 128])
    sel_d = din("sel", [128, 2])
    tmpl_d = din("tmpl", [128, ROW], BF16)

    out_d = nc.dram_tensor("out", [HALF, D], F32, kind="ExternalOutput").ap()
    xn_scr = nc.dram_tensor("xn_scr", [HALF, D], F32, kind="Internal").ap()
    ym_scr = nc.dram_tensor("ym_scr", [HALF, D], BF16, kind="Internal").ap()
    xg_d = nc.dram_tensor("xg_scr", [NEXP * CAP, ROW], BF16, kind="Internal").ap()
    yo_d = nc.dram_tensor("yo_scr", [2 * HALF, D], F32, kind="Internal").ap()
    wout_b = nc.dram_tensor("wout_b", [D, D], BF16, kind="Internal").ap()
    wpg_b = nc.dram_tensor("wpg_b", [D, D], BF16, kind="Internal").ap()
    wpp_b = nc.dram_tensor("wpp_b", [256, D], BF16, kind="Internal").ap()
    weg_b = nc.dram_tensor("weg_b", [NEXP, D, DEXP], BF16, kind="Internal").ap()
    weu_b = nc.dram_tensor("weu_b", [NEXP, D, DEXP], BF16, kind="Internal").ap()
    wed_b = nc.dram_tensor("wed_b", [NEXP, DEXP, D], BF16, kind="Internal").ap()
    dbg_d = None
    if debug:
        dbg_d = nc.dram_tensor("dbg", [HALF, D], F32, kind="ExternalOutput").ap()

    NBYTES = 212480
    big = es.enter_context(nc.sbuf_tensor("big", [128, NBYTES // 2], BF16))
    psum = es.enter_context(nc.psum_tensor("psum", [128, 8, 512], F32))
    P = Prog(nc, es)
    P.reg_req = {"xg": NEXP * CAP - 1, "yo": 2 * HALF - 1}
    ps_rr = [0, 0, 0]
    ps_stream = [0]

    def ps():
        st = ps_stream[0]
        if st == 0:
            b = ps_rr[0]
            ps_rr[0] = (b + 1) % 8
        elif st == 1:
            b = ps_rr[1]
            ps_rr[1] = (b + 1) % 4
        else:
            b = 4 + ps_rr[2]
            ps_rr[2] = (ps_rr[2] + 1) % 4
        return ("ps", b), psum[:, b, :]

    A = Arena(big, NBYTES)
    cm32 = A.alloc([128, 6 * 128], F32)
    identb = A.alloc([128, 128], BF16)
    sel = A.alloc([128, 2], F32)
    flag = A.alloc([128, 1], F32)
    eps_t = A.alloc([128, 2], F32)
    st12 = A.alloc([128, 12], F32)
    mv = A.alloc([128, 2], F32)
    rstd = A.alloc([128, 1], F32)
    s8 = A.alloc([128, 48], F32)
    phase_mark = A.off

    ident = identb
    ident32 = cm32[:, 0:128]
    m_su, m_iu, m_sl = cm32[:, 128:256], cm32[:, 256:384], cm32[:, 384:512]
    tri32 = cm32[:, 512:640]
    m_gm = cm32[:, 640:768]

    P.dma("sp", lambda e: e.dma_start(out=cm32, in_=cm_d), writes=["cm32"])
    P.dma("sp", lambda e: e.dma_start(out=sel, in_=sel_d), writes=["sel"])
    P.dma("sp", lambda e: e.dma_start(out=flag, in_=flag_d), writes=["flag"])
    P.dma("pool", lambda e: e.dma_start(out=identb, in_=cm_d[:, 0:128]), writes=["cmb"])
    P.op("pool", lambda e: e.memset(eps_t[:, 0:1], LN_EPS), writes=["eps"])
    for t in range(NEXP * CAP // 128):
        P.dma("sp", lambda e, t=t: e.dma_start(out=xg_d[t * 128:(t + 1) * 128, :], in_=tmpl_d), writes=["XgT%d" % t])
    P.op("pool", lambda e: e.memset(eps_t[:, 1:2], GN_EPS), writes=["eps"])

    def ln_tile(x, gk, bk, key, vkey):
        P.op("dve", lambda e: e.bn_stats(st12[:, 0:6], x[:, 0:512]), reads=[key], writes=["st12a"])
        P.op("dve", lambda e: e.bn_stats(st12[:, 6:12], x[:, 512:1024]), reads=[key], writes=["st12b"])
        P.op("dve", lambda e: e.bn_aggr(mv, st12), reads=["st12a", "st12b"], writes=["mv"])
        P.op("act", ACTF(rstd, mv[:, 1:2], AF.Sqrt, bias=eps_t[:, 0:1]), reads=["mv", "eps"], writes=["rstd"])
        P.op("dve", lambda e: e.reciprocal(rstd, rstd), reads=["rstd"], writes=["rstd"])
        P.op("dve", TS(x, x, mv[:, 0:1], rstd[:, 0:1], ALU.subtract, ALU.mult), reads=[key, "mv", "rstd"], writes=[key])
        P.op("dve", TT(x, x, gk, ALU.mult), reads=[key, vkey], writes=[key])
        P.op("dve", TT(x, x, bk, ALU.add), reads=[key, vkey], writes=[key])

    def v3(t, h=8):
        return t.rearrange("p (h j) -> p h j", h=h)

    def hv(t, h):
        return t[:, h * 64:(h + 1) * 64]

    def transposes(src, src_keys, n, width=128):
        k, bank = ps()
        pb16 = bank.bitcast(BF16)
        fns = [TR(pb16[0:width, j * 128:(j + 1) * 128], src[:, j * width:(j + 1) * width], ident) for j in range(n)]
        P.op("pe", fns, reads=list(src_keys) + ["cmb"], writes=[k])
        return k, pb16

    Win = A.alloc([128, 8, 2816], BF16)
    wlora = A.alloc([128, 512], BF16)
    wgate = A.alloc([128, 512], BF16)
    wsT = A.alloc([128, 512], BF16)
    bsp = A.alloc([128, 4], F32)
    vec1 = A.alloc([128, NV1], F32)

    def V1(nm):
        o, n = O1[nm]
        return vec1[:, o:o + n]

    def dbl(shape, dt):
        return [A.alloc(shape, dt) for _ in range(2)]

    xt = A.alloc([128, D], F32)
    xnb = A.alloc([128, D], BF16)
    xnT = [A.alloc([128, 8, 129], BF16) for _ in range(2)]
    h32 = A.alloc([128, 1792], F32)
    lor = A.alloc([128, 256], BF16)
    lorT = A.alloc([128, 256], BF16)
    sg = A.alloc([128, 512], F32)
    eW = A.alloc([128, 512], F32)
    eWi = A.alloc([128, 512], F32)
    eWm = A.alloc([128, 512], F32)
    al = A.alloc([128, 512], F32)
    kkn = sg
    kp = A.alloc([128, 512], F32)
    tmp = A.alloc([128, 512], F32)
    tmpB = A.alloc([128, 512], F32)
    g32_ = dbl([128, 512], F32)
    zvn = A.alloc([128, 512], BF16)
    AZ_ = dbl([128, 8, 128], BF16)
    Bt_ = dbl([128, 512], BF16)
    Kt_ = dbl([128, 512], BF16)
    Rt_ = dbl([128, 512], BF16)
    Vb_ = dbl([128, 512], BF16)
    ART_ = dbl([64, 8, 256], BF16)
    BTc_ = dbl([64, 8, 128], BF16)
    KTc_ = dbl([64, 8, 128], BF16)
    Nm_ = dbl([128, 8, 128], BF16)
    NmT_ = dbl([128, 8, 128], BF16)
    BRm_ = dbl([128, 8, 128], BF16)
    KRm_ = dbl([128, 8, 128], BF16)
    KAm_ = dbl([128, 8, 128], BF16)
    Wc_ = dbl([64, 8, 2], F32)
    rkf_ = dbl([128, 8], F32)
    PT3 = A.alloc([128, 8, 384], BF16)
    PTb1 = A.alloc([128, 8, 128], BF16)
    TTm = PT3[:, :, 128:256]
    AV = A.alloc([128, 8, 128], BF16)
    AhT = A.alloc([64, 8, 128], BF16)
    PpT = A.alloc([64, 2, 8, 64], BF16)
    Qp = A.alloc([64, 2, 512], F32)
    N32 = [A.alloc([64, 512], F32) for _ in range(2)]
    N16 = [A.alloc([64, 512], BF16) for _ in range(4)]
    t1 = tmpB[0:64, :]
    Usb = A.alloc([128, 512], BF16)
    yA = A.alloc([128, 512], BF16)
    yB_ = dbl([128, 512], BF16)
    y32 = A.alloc([128, 512], F32)
    zu = A.alloc([128, 512], F32)
    zv = A.alloc([128, 512], F32)
    ZU, ZV, Y32 = "zu", "zv", "y32"
    print("phase1 sbuf bytes", A.off)

    P.dma("sp", lambda e: e.dma_start(out=vec1, in_=vec1_d), writes=["vec1"])
    P.dma("sp", lambda e: e.dma_start(out=bsp, in_=bsp_d), writes=["bsp"])
    P.dma("sp", lambda e: e.dma_start(out=tmp, in_=wsT_d), writes=["tmp"])
    P.dma("pool", lambda e: e.dma_start(out=wlora, in_=wlora_d), writes=["wlora"])
    P.dma("pool", lambda e: e.dma_start(out=wgate, in_=wgate_d), writes=["wgate"])
    for kc in range(8):
        P.dma("pool", lambda e, kc=kc: e.dma_start(out=Win[:, kc, :], in_=w_in[kc * 128:(kc + 1) * 128, :]), writes=["Win"])
    P.op("dve", TT(v3(wsT, 4), v3(tmp, 4), m_gm.unsqueeze(1).to_broadcast([128, 4, 128]), ALU.mult),
         reads=["tmp", "cm32"], writes=["wsT"])
    P.dma("pool", lambda e: e.dma_start(out=wout_b, in_=w_out), writes=["woutb"])
    P.dma("pool", lambda e: e.dma_start(out=wpg_b, in_=w_pg), writes=["wpgb"])
    P.dma("pool", lambda e: e.dma_start(out=wpp_b, in_=w_pp), writes=["wppb"])
    P.op("pool", lambda e: e.memset(N32[0], 0.0), writes=["N32_0"])
    P.op("pool", lambda e: e.memset(N16[0], 0.0), writes=["N16_0"])
    P.op("pool", lambda e: e.memset(xnT[1][:, :, 128:129], 0.0), writes=["xnT1"])

    su_b = m_su.unsqueeze(1).to_broadcast([128, 4, 128])
    iu_b = m_iu.unsqueeze(1).to_broadcast([128, 4, 128])
    sl_b = m_sl.unsqueeze(1).to_broadcast([128, 4, 128])
    state = {"n": 0}
    a_done = [False] * 33
    j_done = [False] * 33
    n_tiles = N_TILES

    def tile_gen(i):
        own = i >= OWN_FROM
        par = i % 2
        K = lambda nm: "%s_%d" % (nm, par)
        g32, AZ, Bt, Kt, Rt, Vb = g32_[par], AZ_[par], Bt_[par], Kt_[par], Rt_[par], Vb_[par]
        ART, BTc, KTc = ART_[par], BTc_[par], KTc_[par]
        yB = yB_[par]
        Nm, NmT, BRm, KRm, KAm, Wc, rkf = Nm_[par], NmT_[par], BRm_[par], KRm_[par], KAm_[par], Wc_[par], rkf_[par]
        cur, prv = xnT[i % 2], xnT[(i + 1) % 2]
        kcur, kprv = "xnT%d" % (i % 2), "xnT%d" % ((i + 1) % 2)
        e_ = i
        P.dma("pool", lambda e: e.dma_start(out=weg_b[e_], in_=w_eg[e_]), writes=[("wegb", e_)])
        P.dma("pool", lambda e: e.dma_start(out=weu_b[e_], in_=w_eu[e_]), writes=[("weub", e_)])
        P.dma("pool", lambda e: e.dma_start(out=wed_b[e_], in_=w_ed[e_]), writes=[("wedb", e_)])
        while i > 0 and not a_done[i - 1]:
            yield
        P.dma("sp", lambda e: e.dma_start(out=xt, in_=xs[i * 128:(i + 1) * 128, :]), writes=["xt"])
        ln_tile(xt, V1("lneg"), V1("lneb"), "xt", "vec1")
        P.op("act", ACTF(xnb, xt, AF.Copy), reads=["xt"], writes=["xnb"])
        if own:
            P.dma("pool", lambda e: e.dma_start(out=xn_scr[(i - OWN_FROM) * 128:(i - OWN_FROM + 1) * 128, :], in_=xt), reads=["xt"], writes=["xnscr"])
        yield
        k, pb16 = transposes(xnb, ["xnb"], 8)
        P.op("act", ACTF(cur[:, :, 1:129], v3(pb16[:, 0:1024]), AF.Copy), reads=[k], writes=[kcur])
        if i == OWN_FROM:
            P.op("act", ACTF(cur[:, :, 0:1], prv[:, :, 128:129], AF.Copy, scale=flag[:, 0:1]), reads=[kprv, "flag"], writes=[kcur])
        else:
            P.op("act", ACTF(cur[:, :, 0:1], prv[:, :, 128:129], AF.Copy), reads=[kprv], writes=[kcur])
        a_done[i] = True
        yield
        for (c0, cn) in ((0, 512), (512, 512), (1024, 512), (1536, 256)):
            if c0 == 0 and not own:
                continue
            k1, b1 = ps()
            k2, b2 = ps()
            P.op("pe", [MM(b1[:, 0:cn], cur[:, kc, 1:129], Win[:, kc, c0:c0 + cn], kc == 0, kc == 7) for kc in range(8)],
                 reads=[kcur, "Win"], writes=[k1])
            P.op("pe", [MM(b2[:, 0:cn], cur[:, kc, 0:128], Win[:, kc, c0:c0 + cn], kc == 0, kc == 7) for kc in range(8)],
                 reads=[kcur, "Win"], writes=[k2])
            hk = "h32_%d" % c0
            hs_ = h32[:, c0:c0 + cn]
            P.op("act", ACTF(hs_, b1[:, 0:cn], AF.Copy), reads=[k1], writes=[hk])
            P.op("dve", TT(tmp[:, 0:cn], b2[:, 0:cn], hs_, ALU.subtract), reads=[k2, hk], writes=["tmp"])
            P.op("dve", TT(tmp[:, 0:cn], tmp[:, 0:cn], V1("mu")[:, c0:c0 + cn], ALU.mult), reads=["tmp", "vec1"], writes=["tmp"])
            P.op("dve", TT(hs_, hs_, tmp[:, 0:cn], ALU.add), reads=["tmp", hk], writes=[hk])
            if not own:
                P.op("dve", TS(hs_, hs_, flag[:, 0:1]), reads=[hk, "flag"], writes=[hk])
            yield
        if own:
            for gi, dst, dk in ((0, zu, ZU), (1, zv, ZV)):
                c0 = 1792 + gi * 512
                kq, bank = ps()
                P.op("pe", [MM(bank, cur[:, kc, 1:129], Win[:, kc, c0:c0 + 512], kc == 0, kc == 7) for kc in range(8)], reads=[kcur, "Win"], writes=[kq])
                P.op("act", ACTF(dst, bank, AF.Gelu), reads=[kq], writes=[dk])
                yield
            zv4 = v3(zv, 4)
            P.op("dve", RED(s8[:, 32:36], zv4), reads=[ZV], writes=["s8e"])
            P.op("dve", TT(tmp, zv, zv, ALU.mult), reads=[ZV], writes=["tmp"])
            P.op("dve", RED(s8[:, 36:40], v3(tmp, 4)), reads=["tmp"], writes=["s8f"])
            P.op("dve", TS(s8[:, 32:36], s8[:, 32:36], 1.0 / 128), reads=["s8e"], writes=["s8e"])
            P.op("dve", TT(s8[:, 40:44], s8[:, 32:36], s8[:, 32:36], ALU.mult), reads=["s8e"], writes=["s8g"])
            P.op("dve", STT(s8[:, 36:40], s8[:, 36:40], 1.0 / 128, s8[:, 40:44], ALU.mult, ALU.subtract), reads=["s8f", "s8g"], writes=["s8f"])
            P.op("act", ACTF(s8[:, 36:40], s8[:, 36:40], AF.Sqrt, bias=eps_t[:, 0:1]), reads=["s8f", "eps"], writes=["s8f"])
            P.op("dve", lambda e: e.reciprocal(s8[:, 36:40], s8[:, 36:40]), reads=["s8f"], writes=["s8f"])
            yield
            P.op("dve", TT(zv4, zv4, s8[:, 32:36].unsqueeze(2).to_broadcast([128, 4, 128]), ALU.subtract), reads=[ZV, "s8e"], writes=[ZV])
            P.op("dve", TT(zv4, zv4, s8[:, 36:40].unsqueeze(2).to_broadcast([128, 4, 128]), ALU.mult), reads=[ZV, "s8f"], writes=[ZV])
            P.op("dve", TT(zv, zv, V1("glg"), ALU.mult), reads=[ZV, "vec1"], writes=[ZV])
            P.op("dve", TT(zvn, zv, V1("glb"), ALU.add), reads=[ZV, "vec1"], writes=["zvn"])
            yield
            kq, bank = ps()
            P.op("pe", [MM(bank[:, g * 128:(g + 1) * 128], wsT[:, g * 128:(g + 1) * 128], zvn[:, g * 128:(g + 1) * 128]) for g in range(4)],
                 reads=["wsT", "zvn"], writes=[kq])
            for g in range(4):
                P.op("dve", STT(yB[:, g * 128:(g + 1) * 128], bank[:, g * 128:(g + 1) * 128], bsp[:, g:g + 1],
                                zu[:, g * 128:(g + 1) * 128], ALU.add, ALU.mult), reads=[kq, "bsp", ZU], writes=[K("yB%d" % g)])
            P.dma("pool", lambda e: e.dma_start(out=ym_scr[(i - OWN_FROM) * 128:(i - OWN_FROM + 1) * 128, 512:1024], in_=yB),
                  reads=[K("yB%d" % g) for g in range(4)], writes=["ymscrB"])
            yield
        r_, k_, v_ = h32[:, 0:512], h32[:, 512:1024], h32[:, 1024:1536]
        P.op("act", ACTF(lor[:, 0:64], h32[:, 1536:1600], AF.Tanh), reads=["h32_1536"], writes=["lor"])
        P.op("act", ACTF(lor[:, 64:128], h32[:, 1600:1664], AF.Copy), reads=["h32_1536"], writes=["lor"])
        P.op("act", ACTF(lor[:, 128:256], h32[:, 1664:1792], AF.Sigmoid), reads=["h32_1536"], writes=["lor"])
        k, pb16 = transposes(lor, ["lor"], 2)
        P.op("act", ACTF(lorT, pb16[:, 0:256], AF.Copy), reads=[k], writes=["lorT"])
        yield
        kd, bd = ps()
        P.op("pe", MM(bd, lorT[0:64, 0:128], wlora[0:64, :]), reads=["lorT", "wlora"], writes=[kd])
        P.op("dve", TT(sg, bd, V1("w0"), ALU.add), reads=[kd, "vec1"], writes=["sg"])
        P.op("act", ACTF(sg, sg, AF.Sigmoid), reads=["sg"], writes=["sg"])
        ka_, ba = ps()
        P.op("pe", MM(ba, lorT[64:128, 0:128], wlora[64:128, :]), reads=["lorT", "wlora"], writes=[ka_])
        P.op("dve", TT(al, ba, V1("a0"), ALU.add), reads=[ka_, "vec1"], writes=["al"])
        P.op("act", ACTF(al, al, AF.Sigmoid), reads=["al"], writes=["al"])
        if own:
            kg, bg = ps()
            P.op("pe", MM(bg, lorT[:, 128:256], wgate), reads=["lorT", "wgate"], writes=[kg])
            P.op("act", ACTF(g32, bg, AF.Copy), reads=[kg], writes=[K("g32")])
        yield
        kc_, bc = ps()
        P.op("pe", MM(bc, tri32, sg), reads=["sg", "cm32"], writes=[kc_])
        P.op("act", ACTF(eW, bc, AF.Exp, scale=-DS), reads=[kc_], writes=["eW"])
        P.op("act", ACTF(eWi, bc, AF.Exp, scale=DS), reads=[kc_], writes=["eWi"])
        P.op("dve", TT(tmp, bc, sg, ALU.subtract), reads=[kc_, "sg", "eW", "eWi"], writes=["tmp"])
        P.op("act", ACTF(eWm, tmp, AF.Exp, scale=-DS), reads=["tmp"], writes=["eWm"])
        yield
        P.op("dve", TT(kkn, k_, V1("kk"), ALU.mult), reads=["h32_512", "vec1"], writes=["sg"])
        P.op("dve", TT(tmp, kkn, kkn, ALU.mult), reads=["sg"], writes=["tmp"])
        P.op("dve", RED(s8[:, 0:8], v3(tmp)), reads=["tmp"], writes=["s8a"])
        P.op("dve", TS(s8[:, 0:8], s8[:, 0:8], 1e-24, None, ALU.max), reads=["s8a"], writes=["s8a"])
        P.op("act", ACTF(s8[:, 0:8], s8[:, 0:8], AF.Sqrt), reads=["s8a"], writes=["s8a"])
        P.op("dve", lambda e: e.reciprocal(s8[:, 0:8], s8[:, 0:8]), reads=["s8a"], writes=["s8a"])
        P.op("dve", TT(v3(kkn), v3(kkn), s8[:, 0:8].unsqueeze(2).to_broadcast([128, 8, 64]), ALU.mult), reads=["sg", "s8a"], writes=["sg"])
        yield
        P.op("dve", STT(kp, al, -1.0, V1("ka"), ALU.add, ALU.mult), reads=["al", "vec1"], writes=["kp"])
        P.op("dve", STT(kp, kp, 1.0, k_, ALU.add, ALU.mult), reads=["kp", "h32_512"], writes=["kp"])
        P.op("dve", STT(AZ[:, :, 0:64], v3(kkn), -1.0, v3(eWm), ALU.mult, ALU.mult), reads=["sg", "eWm"], writes=[K("AZa")])
        P.op("dve", TT(tmp, kkn, al, ALU.mult), reads=["sg", "al"], writes=["tmp"])
        P.op("dve", TT(Bt, tmp, eWi, ALU.mult), reads=["tmp", "eWi"], writes=[K("Bt")])
        P.op("dve", TT(Kt, kp, eWi, ALU.mult), reads=["kp", "eWi"], writes=[K("Kt")])
        P.op("act", ACTF(Vb, v_, AF.Copy), reads=["h32_1024"], writes=[K("Vb")])
        yield
        if own:
            P.op("dve", TT(Rt, r_, eW, ALU.mult), reads=["h32_0", "eW"], writes=[K("Rt")])
            P.op("dve", TT(tmp, r_, kp, ALU.mult), reads=["h32_0", "kp"], writes=["tmp"])
            P.op("dve", TT(tmp, tmp, V1("rk"), ALU.mult), reads=["tmp", "vec1"], writes=["tmp"])
            P.op("dve", RED(rkf, v3(tmp)), reads=["tmp"], writes=[K("rkf")])
        kq, bank = ps()
        P.op("pe", [MM(bank[0:64, h * 2:h * 2 + 2], hv(eW, h), sel) for h in range(8)], reads=["eW", "sel"], writes=[kq])
        P.op("act", ACTF(Wc, v3(bank[0:64, 0:16]), AF.Copy), reads=[kq], writes=[K("Wc")])
        yield
        def tr_heads(srcf, skey, dst, dkey):
            kq, bank = ps()
            pq = bank.bitcast(BF16)
            P.op("pe", [TR(pq[0:64, h * 128:(h + 1) * 128], srcf(h), ident) for h in range(8)], reads=[skey, "cmb"], writes=[kq])
            P.op("act", ACTF(dst, v3(pq[0:64, :]), AF.Copy), reads=[kq], writes=[dkey])
        tr_heads(lambda h: AZ[:, h, 0:64], K("AZa"), ART[:, :, 0:128], K("ARTa"))
        yield
        if own:
            tr_heads(lambda h: hv(Rt, h), K("Rt"), ART[:, :, 128:256], K("ARTr"))
            yield
        tr_heads(lambda h: hv(Bt, h), K("Bt"), BTc, K("BTc"))
        yield
        tr_heads(lambda h: hv(Kt, h), K("Kt"), KTc, K("KTc"))
        yield
        def lmat(lf, rf, rkeys, mask_b, dst, dkey):
            for hg in range(2):
                kq, bank = ps()
                b3 = v3(bank, 4)
                P.op("pe", [MM(b3[:, h - hg * 4, :], lf(h), rf(h)) for h in range(hg * 4, hg * 4 + 4)], reads=rkeys, writes=[kq])
                P.op("dve", TT(dst[:, hg * 4:hg * 4 + 4, :], b3, mask_b, ALU.mult), reads=[kq, "cm32"], writes=[dkey])
        lmat(lambda h: BTc[:, h, :], lambda h: ART[:, h, 0:128], [K("BTc"), K("ARTa")], su_b, Nm, K("Nm"))
        yield
        lmat(lambda h: ART[:, h, 0:128], lambda h: BTc[:, h, :], [K("BTc"), K("ARTa")], sl_b, NmT, K("NmT"))
        yield
        lmat(lambda h: KTc[:, h, :], lambda h: ART[:, h, 0:128], [K("KTc"), K("ARTa")], su_b, KAm, K("KAm"))
        yield
        if own:
            lmat(lambda h: BTc[:, h, :], lambda h: ART[:, h, 128:256], [K("BTc"), K("ARTr")], iu_b, BRm, K("BRm"))
            yield
            lmat(lambda h: KTc[:, h, :], lambda h: ART[:, h, 128:256], [K("KTc"), K("ARTr")], iu_b, KRm, K("KRm"))
            yield
        yield "HALF"
        if DBG_STOP == "E":
            return
        SA, ST, SB = slice(0, 128), slice(128, 256), slice(256, 384)
        P.op("dve", TT(PT3[:, 0:4, ST], Nm[:, 0:4, :], ident32.unsqueeze(1).to_broadcast([128, 4, 128]), ALU.add), reads=[K("Nm"), "cm32"], writes=["T0"])
        P.op("dve", TT(PT3[:, 4:8, ST], Nm[:, 4:8, :], ident32.unsqueeze(1).to_broadcast([128, 4, 128]), ALU.add), reads=[K("Nm"), "cm32"], writes=["T1"])
        for hg in range(2):
            hs = range(hg * 4, hg * 4 + 4)
            kq, bank = ps()
            b3 = v3(bank, 4)
            P.op("pe", [MM(b3[:, h - hg * 4, :], NmT[:, h, :], Nm[:, h, :]) for h in hs], reads=[K("Nm"), K("NmT")], writes=[kq])
            P.op("act", ACTF(PT3[:, hg * 4:hg * 4 + 4, SB], b3, AF.Copy), reads=[kq], writes=["PB%d" % hg])
            kq, bank = ps()
            b3 = v3(bank, 4)
            P.op("pe", [MM(b3[:, h - hg * 4, :], Nm[:, h, :], NmT[:, h, :]) for h in hs], reads=[K("Nm"), K("NmT")], writes=[kq])
            P.op("act", ACTF(PTb1[:, hg * 4:hg * 4 + 4, :], b3, AF.Copy), reads=[kq], writes=["PTb1_%d" % hg])
            yield
        if DBG_STOP == "F1":
            return
        p_in_b = True
        pt_in_1 = True
        for lev in range(2, 6):
            if DBG_STOP == "F%d" % lev:
                return
            for hg in range(2):
                hs = range(hg * 4, hg * 4 + 4)
                hsl_ = slice(hg * 4, hg * 4 + 4)
                PTc = PTb1 if pt_in_1 else NmT
                kPTc = ("PTb1_%d" % hg) if pt_in_1 else K("NmT")
                PTn = NmT if pt_in_1 else PTb1
                kPTn = K("NmT") if pt_in_1 else ("PTb1_%d" % hg)
                kPc = ("PB%d" % hg) if p_in_b else ("PA%d" % hg)
                kPn = ("PA%d" % hg) if p_in_b else ("PB%d" % hg)
                Pc_s = SB if p_in_b else SA
                Pn_s = SA if p_in_b else SB
                kT = "T%d" % hg
                extra = []
                if lev < 5:
                    rsl = slice(128, 384) if p_in_b else slice(0, 256)
                    xo = 0 if p_in_b else 128
                    po = 128 if p_in_b else 0
                    kq1, bank1 = ps()
                    kq2, bank2 = ps()
                    fns = []
                    for h in hs:
                        bk = bank1 if (h - hg * 4) < 2 else bank2
                        o = ((h - hg * 4) % 2) * 256
                        fns.append(MM(bk[:, o:o + 256], PTc[:, h, :], PT3[:, h, rsl]))
                    P.op("pe", fns, reads=[kPTc, kPc, kT] + extra, writes=[kq1, kq2])
                    if DBG_STOP == "L2a":
                        return
                    for bi, (kq_, bk) in enumerate(((kq1, bank1), (kq2, bank2))):
                        bb = bk.rearrange("p (h t) -> p h t", h=2)
                        hh = slice(hg * 4 + bi * 2, hg * 4 + bi * 2 + 2)
                        if L2_DVE:
                            P.op("dve", TT(PT3[:, hh, ST], PT3[:, hh, ST], bb[:, :, xo:xo + 128], ALU.add), reads=[kq_, kT], writes=[kT])
                            P.op("dve", CP(PT3[:, hh, Pn_s], bb[:, :, po:po + 128]), reads=[kq_], writes=[kPn])
                        else:
                            P.op("act", ACTF(PT3[:, hh, Pn_s], bb[:, :, po:po + 128], AF.Copy), reads=[kq_], writes=[kPn])
                            P.op("dve", TT(PT3[:, hh, ST], PT3[:, hh, ST], bb[:, :, xo:xo + 128], ALU.add), reads=[kq_, kT, kPn], writes=[kT])
                else:
                    kq, bank = ps()
                    b3 = v3(bank, 4)
                    P.op("pe", [MM(b3[:, h - hg * 4, :], PTc[:, h, :], PT3[:, h, ST]) for h in hs], reads=[kPTc, kT], writes=[kq])
                    P.op("dve", TT(PT3[:, hsl_, ST], PT3[:, hsl_, ST], b3, ALU.add), reads=[kq, kT], writes=[kT])
                kq, bank = ps()
                b3 = v3(bank, 4)
                P.op("pe", [MM(b3[:, h - hg * 4, :], PT3[:, h, Pc_s], PTc[:, h, :]) for h in hs], reads=[kPTc, kPc] + extra, writes=[kq])
                P.op("act", ACTF(PTn[:, hsl_, :], b3, AF.Copy), reads=[kq] + extra, writes=[kPTn])
                yield
            p_in_b = not p_in_b
            pt_in_1 = not pt_in_1
        for hg in range(2):
            hs = range(hg * 4, hg * 4 + 4)
            hsl_ = slice(hg * 4, hg * 4 + 4)
            PTc = PTb1 if pt_in_1 else NmT
            kPTc = ("PTb1_%d" % hg) if pt_in_1 else K("NmT")
            kT = "T%d" % hg
            kq, bank = ps()
            b3 = v3(bank, 4)
            P.op("pe", [MM(b3[:, h - hg * 4, :], PTc[:, h, :], PT3[:, h, ST]) for h in hs], reads=[kPTc, kT], writes=[kq])
            P.op("dve", TT(PT3[:, hsl_, ST], PT3[:, hsl_, ST], b3, ALU.add), reads=[kq, kT], writes=[kT])
            yield
        if DBG_STOP == "F":
            return
        kq, bank = ps()
        P.op("pe", [MM(bank[:, h * 64:(h + 1) * 64], KAm[:, h, :], hv(Vb, h)) for h in range(8)], reads=[K("KAm"), K("Vb")], writes=[kq])
        P.op("act", ACTF(AZ[:, :, 64:128], v3(bank), AF.Copy), reads=[kq], writes=[K("AZz")])
        yield
        for hg in range(2):
            kq, bank = ps()
            b3 = v3(bank, 4)
            P.op("pe", [MM(b3[:, h - hg * 4, :], TTm[:, h, :], AZ[:, h, :]) for h in range(hg * 4, hg * 4 + 4)], reads=["T%d" % hg, K("AZa"), K("AZz")], writes=[kq])
            P.op("act", ACTF(AV[:, hg * 4:hg * 4 + 4, :], b3, AF.Copy), reads=[kq], writes=["AV"])
            yield
        if own:
            for hg in range(2):
                kq, bank = ps()
                b3 = v3(bank, 4)
                P.op("pe", [MM(b3[0:64, h - hg * 4, :], AZ[:, h, 0:64], TTm[:, h, :]) for h in range(hg * 4, hg * 4 + 4)], reads=["T%d" % hg, K("AZa")], writes=[kq])
                P.op("act", ACTF(AhT[:, hg * 4:hg * 4 + 4, :], b3[0:64], AF.Copy), reads=[kq], writes=["AhT"])
                yield
        for c in range(2):
            pb = 64 * c
            kq, bank = ps()
            P.op("pe", [MM(bank[0:64, h * 64:(h + 1) * 64], AV[pb:pb + 64, h, 0:64], Bt[pb:pb + 64, h * 64:(h + 1) * 64]) for h in range(8)],
                 reads=["AV", K("Bt")], writes=[kq])
            P.op("act", ACTF(PpT[:, c, :, :], v3(bank[0:64, :]), AF.Copy), reads=[kq], writes=["PpT%d" % c])
            kq, bank = ps()
            fns = []
            for h in range(8):
                fns.append(MM(bank[0:64, h * 64:(h + 1) * 64], Bt[pb:pb + 64, h * 64:(h + 1) * 64], AV[pb:pb + 64, h, 64:128], True, False))
                fns.append(MM(bank[0:64, h * 64:(h + 1) * 64], Kt[pb:pb + 64, h * 64:(h + 1) * 64], Vb[pb:pb + 64, h * 64:(h + 1) * 64], False, True))
            P.op("pe", fns, reads=["AV", K("Bt"), K("Kt"), K("Vb")], writes=[kq])
            P.op("act", ACTF(Qp[:, c, :], bank[0:64, :], AF.Copy), reads=[kq], writes=["Qp%d" % c])
            yield
        while i > 0 and not j_done[i - 1]:
            yield
        st_in = []
        for c in range(2):
            nstate = state["n"]
            n16c, n32c = N16[nstate % 4], N32[nstate % 2]
            k16c, k32c = "N16_%d" % (nstate % 4), "N32_%d" % (nstate % 2)
            n16n, n32n = N16[(nstate + 1) % 4], N32[(nstate + 1) % 2]
            k16n, k32n = "N16_%d" % ((nstate + 1) % 4), "N32_%d" % ((nstate + 1) % 2)
            st_in.append((n16c, k16c))
            kq, bank = ps()
            P.op("pe", [MM(bank[0:64, h * 64:(h + 1) * 64], PpT[:, c, h, :], n16c[:, h * 64:(h + 1) * 64]) for h in range(8)],
                 reads=["PpT%d" % c, k16c], writes=[kq])
            P.op("dve", TT(t1, bank[0:64, :], n32c, ALU.add), reads=[kq, k32c], writes=["tmpB"])
            P.op("dve", TT(t1, t1, Qp[:, c, :], ALU.add), reads=["tmpB", "Qp%d" % c], writes=["tmpB"])
            P.op("dve", TT(v3(n32n), v3(t1), Wc[:, :, c:c + 1].to_broadcast([64, 8, 64]), ALU.mult), reads=["tmpB", K("Wc")], writes=[k32n])
            P.op("act", ACTF(n16n, n32n, AF.Copy), reads=[k32n], writes=[k16n])
            state["n"] += 1
        j_done[i] = True
        yield
        if not own:
            return
        kq, bank = ps()
        fns = []
        for c in range(2):
            pb = 64 * c
            for h in range(8):
                fns.append(MM(bank[pb:pb + 64, h * 64:(h + 1) * 64], AhT[:, h, pb:pb + 64], st_in[c][0][:, h * 64:(h + 1) * 64]))
        P.op("pe", fns, reads=["AhT", st_in[0][1], st_in[1][1]], writes=[kq])
        P.op("dve", TT(v3(Usb), v3(bank), AV[:, :, 64:128], ALU.add), reads=[kq, "AV"], writes=["Usb"])
        yield
        kq, bank = ps()
        fns = []
        for h in range(8):
            for c in range(2):
                pb = 64 * c
                fns.append(MM(bank[pb:pb + 64, h * 64:(h + 1) * 64], ART[:, h, 128 + pb:128 + pb + 64], st_in[c][0][:, h * 64:(h + 1) * 64], True, False))
            fns.append(MM(bank[:, h * 64:(h + 1) * 64], BRm[:, h, :], hv(Usb, h), False, False))
            fns.append(MM(bank[:, h * 64:(h + 1) * 64], KRm[:, h, :], hv(Vb, h), False, True))
        P.op("pe", fns, reads=[K("ARTr"), st_in[0][1], st_in[1][1], K("BRm"), K("KRm"), "Usb", K("Vb")], writes=[kq])
        P.op("act", ACTF(y32, bank, AF.Copy), reads=[kq], writes=[Y32])
        yield
        P.op("dve", RED(s8[:, 8:16], v3(y32)), reads=[Y32], writes=["s8b"])
        P.op("dve", TT(tmpB, y32, y32, ALU.mult), reads=[Y32], writes=["tmpB"])
        P.op("dve", RED(s8[:, 16:24], v3(tmpB)), reads=["tmpB"], writes=["s8c"])
        P.op("dve", TS(s8[:, 8:16], s8[:, 8:16], 1.0 / 64), reads=["s8b"], writes=["s8b"])
        P.op("dve", TT(s8[:, 24:32], s8[:, 8:16], s8[:, 8:16], ALU.mult), reads=["s8b"], writes=["s8d"])
        P.op("dve", STT(s8[:, 16:24], s8[:, 16:24], 1.0 / 64, s8[:, 24:32], ALU.mult, ALU.subtract), reads=["s8c", "s8d"], writes=["s8c"])
        P.op("act", ACTF(s8[:, 16:24], s8[:, 16:24], AF.Sqrt, bias=eps_t[:, 1:2]), reads=["s8c", "eps"], writes=["s8c"])
        P.op("dve", lambda e: e.reciprocal(s8[:, 16:24], s8[:, 16:24]), reads=["s8c"], writes=["s8c"])
        yield
        P.op("dve", TT(v3(y32), v3(y32), s8[:, 8:16].unsqueeze(2).to_broadcast([128, 8, 64]), ALU.subtract), reads=[Y32, "s8b"], writes=[Y32])
        P.op("dve", TT(v3(y32), v3(y32), s8[:, 16:24].unsqueeze(2).to_broadcast([128, 8, 64]), ALU.mult), reads=[Y32, "s8c"], writes=[Y32])
        P.op("dve", TT(y32, y32, V1("gng"), ALU.mult), reads=[Y32, "vec1"], writes=[Y32])
        P.op("dve", TT(y32, y32, V1("gnb"), ALU.add), reads=[Y32, "vec1"], writes=[Y32])
        P.op("dve", TT(v3(tmpB), v3(Vb), rkf.unsqueeze(2).to_broadcast([128, 8, 64]), ALU.mult), reads=[K("Vb"), K("rkf")], writes=["tmpB"])
        P.op("dve", TT(y32, y32, tmpB, ALU.add), reads=[Y32, "tmpB"], writes=[Y32])
        P.op("dve", TT(yA, y32, g32, ALU.mult), reads=[Y32, K("g32")], writes=["yA"])
        P.dma("pool", lambda e: e.dma_start(out=ym_scr[(i - OWN_FROM) * 128:(i - OWN_FROM + 1) * 128, 0:512], in_=yA), reads=["yA"], writes=["ymscrA"])
        yield
    def step(g, stream=0):
        ps_stream[0] = stream
        try:
            return next(g), False
        except StopIteration:
            return None, True
        finally:
            ps_stream[0] = 0

    def drive(gen_fn, n_items):
        if not PIPELINE:
            for i in range(n_items):
                for _ in gen_fn(i):
                    pass
            return
        nxt = 1
        a, a_back, b, b_half = gen_fn(0), False, None, False
        while a is not None:
            r, fin = step(a, 2 if a_back else 1)
            if fin:
                a, a_back, b, b_half = b, b_half, None, False
                if a is not None and a_back and nxt < n_items:
                    b = gen_fn(nxt)
                    nxt += 1
                elif a is None and nxt < n_items:
                    a = gen_fn(nxt)
                    nxt += 1
                continue
            if r == "HALF":
                a_back = True
                if b is None and nxt < n_items:
                    b = gen_fn(nxt)
                    nxt += 1
            if b is not None and not b_half:
                r, fin = step(b, 1)
                assert not fin
                if r == "HALF":
                    b_half = True

    drive(tile_gen, n_tiles)

    if debug == "ym":
        return nc, P, es
    P.barrier()
    A.off = phase_mark
    vec2 = A.alloc([128, NV2], F32)

    def V2(nm):
        o, n = O2[nm]
        return vec2[:, o:o + n]

    acc = A.alloc([128, NT_OWN, D], F32)
    wreg = A.alloc([128, 24 * 1024], BF16)
    Wout = wreg[:, 0:8192].rearrange("p (k n) -> p k n", k=8)
    Wpg = wreg[:, 8192:16384].rearrange("p (k n) -> p k n", k=8)
    Wpp = wreg[:, 16384:18432].rearrange("p (k n) -> p k n", k=2)
    wrt = A.alloc([128, 8, 36], BF16)
    sgmb = A.alloc([128, 128], BF16)
    onesb = A.alloc([128, 128], BF16)
    base = A.alloc([128, 32], F32)
    mark2 = A.off
    ymt_ = [A.alloc([128, D], BF16) for _ in range(2)]
    ymT = A.alloc([128, 8, 128], BF16)
    xnt_ = [A.alloc([128, D], F32) for _ in range(2)]
    rowb = [A.alloc([128, ROW], BF16) for _ in range(4)]
    x1Tt_ = [A.alloc([128, 8, 128], BF16) for _ in range(2)]
    ptl_ = [A.alloc([128, 256], F32) for _ in range(2)]
    pbf = A.alloc([128, 256], BF16)
    pT = A.alloc([128, 2, 128], BF16)
    sgt = A.alloc([128, 512], F32)
    lg = A.alloc([128, 36], F32)
    r8 = A.alloc([128, 64], F32)
    selb = A.alloc([128, 32], BF16)
    rsc = A.alloc([128, 32], F32)
    rt32 = A.alloc([128, 4, 32], F32)
    idxi = [A.alloc([128, 2], I32) for _ in range(2)]
    print("phase2a sbuf bytes", A.off)

    P.dma("sp", lambda e: e.dma_start(out=vec2, in_=vec2_d), writes=["vec2"])
    P.dma("sp", lambda e: e.dma_start(out=Wout, in_=wout_b.rearrange("(k p) n -> p k n", p=128)), reads=["woutb"], writes=["Wout"])
    P.dma("sp", lambda e: e.dma_start(out=Wpg, in_=wpg_b.rearrange("(k p) n -> p k n", p=128)), reads=["wpgb"], writes=["Wpg"])
    P.dma("sp", lambda e: e.dma_start(out=Wpp, in_=wpp_b.rearrange("(k p) n -> p k n", p=128)), reads=["wppb"], writes=["Wpp"])
    P.dma("pool", lambda e: e.dma_start(out=wrt, in_=wrt_d.rearrange("(k p) n -> p k n", p=128)), writes=["wrt"])
    P.op("dve", TT(sgmb, m_gm, ident32, ALU.subtract), reads=["cm32"], writes=["sgmb"])
    P.op("pool", lambda e: e.memset(onesb, 1.0), writes=["onesb"])
    P.op("pool", lambda e: e.memset(base, 0.0), writes=["base"])

    def pre_gen(i):
        ymt, xnt, x1Tt, ptl = ymt_[i % 2], xnt_[i % 2], x1Tt_[i % 2], ptl_[i % 2]
        Kp = lambda nm: "%s_%d" % (nm, i % 2)
        P.dma("sp", lambda e: e.dma_start(out=ymt, in_=ym_scr[i * 128:(i + 1) * 128, :]), reads=["ymscrA", "ymscrB"], writes=[Kp("ymt")])
        P.dma("sp", lambda e: e.dma_start(out=xnt, in_=xn_scr[i * 128:(i + 1) * 128, :]), reads=["xnscr"], writes=[Kp("xnt")])
        P.dma("sp", lambda e: e.dma_start(out=ptl, in_=pin[i * 128:(i + 1) * 128, :]), writes=[Kp("ptl")])
        k, pb16 = transposes(ymt, [Kp("ymt")], 8)
        P.op("act", ACTF(ymT, v3(pb16[:, 0:1024]), AF.Copy), reads=[k], writes=["ymT"])
        for hf in range(2):
            kq, bank = ps()
            P.op("pe", [MM(bank, ymT[:, kc, :], Wout[:, kc, hf * 512:(hf + 1) * 512], kc == 0, kc == 7) for kc in range(8)],
                 reads=["ymT", "Wout"], writes=[kq])
            P.op("dve", STT(xnt[:, hf * 512:(hf + 1) * 512], xnt[:, hf * 512:(hf + 1) * 512], ALPHA, bank, ALU.mult, ALU.add),
                 reads=[kq, Kp("xnt")], writes=[Kp("xnt")])
        yield
        ln_tile(xnt, V2("l1g"), V2("l1b"), Kp("xnt"), "vec2")
        yield
        if debug == "x1":
            P.dma("sp", lambda e: e.dma_start(out=dbg_d[i * 128:(i + 1) * 128, :], in_=xnt), reads=[Kp("xnt")], writes=["dbg"])
        ka = ("acc", i)
        r0, r1 = rowb[(2 * i) % 4], rowb[(2 * i + 1) % 4]
        kr0, kr1 = "rowb%d" % ((2 * i) % 4), "rowb%d" % ((2 * i + 1) % 4)
        P.op("act", ACTF(acc[:, i, :], xnt, AF.Copy, scale=ALPHA), reads=[Kp("xnt")], writes=[ka])
        P.op("act", ACTF(r0[:, 0:1024], xnt, AF.Copy), reads=[Kp("xnt")], writes=[kr0])
        P.op("act", ACTF(r1[:, 0:1024], xnt, AF.Copy), reads=[Kp("xnt")], writes=[kr1])
        k, pb16 = transposes(r0[:, 0:1024], [kr0], 8)
        P.op("act", ACTF(x1Tt, v3(pb16[:, 0:1024]), AF.Copy), reads=[k], writes=[Kp("x1Tt")])
        yield
        P.op("act", ACTF(pbf, ptl, AF.Copy), reads=[Kp("ptl")], writes=["pbf"])
        k, pb16 = transposes(pbf, ["pbf"], 2)
        P.op("act", ACTF(pT, v3(pb16[:, 0:256], 2), AF.Copy), reads=[k], writes=["pT"])
        for hf in range(2):
            cs = slice(hf * 512, (hf + 1) * 512)
            kq, bank = ps()
            P.op("pe", [MM(bank, x1Tt[:, kc, :], Wpg[:, kc, cs], kc == 0, kc == 7) for kc in range(8)], reads=[Kp("x1Tt"), "Wpg"], writes=[kq])
            P.op("dve", TT(sgt, bank, V2("bpg")[:, cs], ALU.add), reads=[kq, "vec2"], writes=["sgt"])
            P.op("act", ACTF(sgt, sgt, AF.Sigmoid), reads=["sgt"], writes=["sgt"])
            kq, bank = ps()
            P.op("pe", [MM(bank, pT[:, kc, :], Wpp[:, kc, cs], kc == 0, kc == 1) for kc in range(2)], reads=["pT", "Wpp"], writes=[kq])
            P.op("dve", TT(sgt, sgt, bank, ALU.mult), reads=[kq, "sgt"], writes=["sgt"])
            P.op("dve", TT(acc[:, i, cs], acc[:, i, cs], sgt, ALU.add), reads=["sgt", ka], writes=[ka])
            yield
        yield "HALF"
        kq, bank = ps()
        P.op("pe", [MM(bank[:, 0:36], x1Tt[:, kc, :], wrt[:, kc, :], kc == 0, kc == 7) for kc in range(8)], reads=[Kp("x1Tt"), "wrt"], writes=[kq])
        P.op("dve", TT(lg, bank[:, 0:36], V2("brt"), ALU.add), reads=[kq, "vec2"], writes=["lg"])
        gl = lg[:, 0:4]
        gmax, ngmax, gsum, pg = r8[:, 0:1], r8[:, 1:2], r8[:, 2:3], r8[:, 3:4]
        oh, gex = r8[:, 4:8], r8[:, 8:12]
        el, m1, m2 = r8[:, 12:20], r8[:, 20:21], r8[:, 21:22]
        mk1, mk2, el2 = r8[:, 24:32], r8[:, 32:40], r8[:, 40:48]
        dm, w1, w2 = r8[:, 22:23], r8[:, 23:24], r8[:, 48:49]
        idf = r8[:, 50:52]
        P.op("dve", RED(gmax, gl, ALU.max), reads=["lg"], writes=["r_gmax"])
        P.op("dve", TS(oh, gl, gmax, None, ALU.is_equal), reads=["lg", "r_gmax"], writes=["r_oh"])
        P.op("dve", TS(ngmax, gmax, -1.0), reads=["r_gmax"], writes=["r_ngmax"])
        P.op("act", ACTF(gex, gl, AF.Exp, bias=ngmax), reads=["lg", "r_ngmax"], writes=["r_gex"])
        P.op("dve", RED(gsum, gex), reads=["r_gex"], writes=["r_gsum"])
        P.op("dve", lambda e: e.reciprocal(pg, gsum), reads=["r_gsum"], writes=["r_pg"])
        yield
        P.op("dve", TT(v3(rsc, 4), v3(lg[:, 4:36], 4), oh.unsqueeze(2).to_broadcast([128, 4, 8]), ALU.mult), reads=["lg", "r_oh"], writes=["rsc"])
        P.op("dve", RED(el, rsc.rearrange("p (g e) -> p e g", g=4)), reads=["rsc"], writes=["r_el"])
        P.op("dve", RED(m1, el, ALU.max), reads=["r_el"], writes=["r_m1"])
        P.op("dve", TS(mk1, el, m1, None, ALU.is_equal), reads=["r_el", "r_m1"], writes=["r_mk1"])
        P.op("dve", STT(el2, mk1, -1e30, el, ALU.mult, ALU.add), reads=["r_mk1", "r_el"], writes=["r_el2"])
        P.op("dve", RED(m2, el2, ALU.max), reads=["r_el2"], writes=["r_m2"])
        P.op("dve", TS(mk2, el2, m2, None, ALU.is_equal), reads=["r_el2", "r_m2"], writes=["r_mk2"])
        P.op("dve", TT(dm, m2, m1, ALU.subtract), reads=["r_m1", "r_m2"], writes=["r_dm"])
        P.op("act", ACTF(dm, dm, AF.Exp), reads=["r_dm"], writes=["r_dm"])
        P.op("dve", TS(w1, dm, 1.0, None, ALU.add), reads=["r_dm"], writes=["r_w1"])
        P.op("dve", lambda e: e.reciprocal(w1, w1), reads=["r_w1"], writes=["r_w1"])
        P.op("dve", TT(w2, dm, w1, ALU.mult), reads=["r_dm", "r_w1"], writes=["r_w2"])
        yield
        P.op("dve", TT(r0[:, 1024:1026].bitcast(F32), w1, pg, ALU.mult), reads=["r_w1", "r_pg", kr0], writes=[kr0])
        P.op("dve", TT(r1[:, 1024:1026].bitcast(F32), w2, pg, ALU.mult), reads=["r_w2", "r_pg", kr1], writes=[kr1])
        P.op("dve", TS(r0[:, 1026:1028].bitcast(I32), V2("pid"), 2.0, float(256 * i), ALU.mult, ALU.add), reads=["vec2", kr0], writes=[kr0])
        P.op("dve", TS(r1[:, 1026:1028].bitcast(I32), V2("pid"), 2.0, float(256 * i + 1), ALU.mult, ALU.add), reads=["vec2", kr1], writes=[kr1])
        s1, s2, sany, posn = rt32[:, 0, :], rt32[:, 1, :], rt32[:, 2, :], rt32[:, 3, :]
        P.op("dve", TT(v3(s1, 4), oh.unsqueeze(2).to_broadcast([128, 4, 8]), mk1.unsqueeze(1).to_broadcast([128, 4, 8]), ALU.mult), reads=["r_oh", "r_mk1"], writes=["s1"])
        P.op("dve", TT(v3(s2, 4), oh.unsqueeze(2).to_broadcast([128, 4, 8]), mk2.unsqueeze(1).to_broadcast([128, 4, 8]), ALU.mult), reads=["r_oh", "r_mk2"], writes=["s2"])
        P.op("dve", TT(selb, s1, s2, ALU.add), reads=["s1", "s2"], writes=["selb"])
        kq, bank = ps()
        P.op("pe", MM(bank[:, 0:32], sgmb, selb), reads=["sgmb", "selb"], writes=[kq])
        P.op("dve", TT(posn, bank[:, 0:32], base, ALU.add), reads=[kq, "base"], writes=["posn"])
        kq, bank = ps()
        P.op("pe", MM(bank[:, 0:32], onesb, selb), reads=["onesb", "selb"], writes=[kq])
        P.op("dve", TT(base, base, bank[:, 0:32], ALU.add), reads=[kq, "base", "posn"], writes=["base"])
        yield
        P.op("dve", TS(sany, posn, float(CAP), 1e6, ALU.is_ge, ALU.mult), reads=["posn"], writes=["sany"])
        P.op("dve", TT(posn, posn, sany, ALU.add), reads=["posn", "sany"], writes=["posn"])
        P.op("dve", TT(posn, posn, V2("eC"), ALU.add), reads=["posn", "vec2"], writes=["posn"])
        P.op("dve", TT(s1, s1, posn, ALU.mult), reads=["s1", "posn"], writes=["s1"])
        P.op("dve", TT(s2, s2, posn, ALU.mult), reads=["s2", "posn"], writes=["s2"])
        P.op("dve", RED(idf, rt32[:, 0:2, :]), reads=["s1", "s2"], writes=["idf"])
        ii = idxi[i % 2]
        kii = "idxi%d" % (i % 2)
        P.op("dve", CP(ii, idf), reads=["idf"], writes=[kii])
        for kk_, (rr, krr) in enumerate(((r0, kr0), (r1, kr1))):
            P.dma("pool", lambda e, rr=rr, ii=ii, kk_=kk_: e.indirect_dma_start(
                out=xg_d[:, :], out_offset=bass.IndirectOffsetOnAxis(ap=ii[:, kk_:kk_ + 1], axis=0), in_=rr[:, :], in_offset=None,
                bounds_check=P.regs["xg"], oob_is_err=False),
                reads=[krr, kii] + ["XgT%d" % t for t in range(NEXP * CAP // 128)], writes=[("Xgs", i, kk_)])

    drive(pre_gen, NT_OWN)

    P.barrier()
    A.off = mark2
    NST = CAP // 128
    xgs = [A.alloc([128, NST, ROW], BF16) for _ in range(2)]
    xgT = [A.alloc([128, 8, CAP], BF16) for _ in range(2)]
    hidb = [A.alloc([128, 4, CAP], BF16) for _ in range(2)]
    hsl = A.alloc([128, CAP], F32)
    yw = [A.alloc([128, D], F32) for _ in range(4)]
    yld = [A.alloc([128, 2, D], F32) for _ in range(2)]
    print("phase2b sbuf bytes", A.off)
    wbufs = []
    for b in range(2):
        o = b * 12288
        wbufs.append((wreg[:, o:o + 4096].rearrange("p (k n) -> p k n", k=8),
                      wreg[:, o + 4096:o + 8192].rearrange("p (k n) -> p k n", k=8),
                      wreg[:, o + 8192:o + 12288].rearrange("p (k n) -> p k n", k=4)))
    ywi = 0
    for e_ in range(NEXP):
        Wg, Wu, Wd = wbufs[e_ % 2]
        kw = "we%d" % (e_ % 2)
        P.dma("sp", lambda e, e_=e_, Wg=Wg: e.dma_start(out=Wg, in_=weg_b[e_].rearrange("(k p) n -> p k n", p=128)), reads=[("wegb", e_)], writes=[kw + "g"])
        P.dma("sp", lambda e, e_=e_, Wu=Wu: e.dma_start(out=Wu, in_=weu_b[e_].rearrange("(k p) n -> p k n", p=128)), reads=[("weub", e_)], writes=[kw + "u"])
        P.dma("sp", lambda e, e_=e_, Wd=Wd: e.dma_start(out=Wd, in_=wed_b[e_].rearrange("(k p) n -> p k n", p=128)), reads=[("wedb", e_)], writes=[kw + "d"])
        xg, kxg = xgs[e_ % 2], "xgs%d" % (e_ % 2)
        xT, kxT = xgT[e_ % 2], "xgT%d" % (e_ % 2)
        hb, khb = hidb[e_ % 2], "hid%d" % (e_ % 2)
        P.dma("sp", lambda e, e_=e_, xg=xg: e.dma_start(out=xg, in_=xg_d[e_ * CAP:(e_ + 1) * CAP, :].rearrange("(t p) r -> p t r", p=128)),
              writes=[kxg])
        for st in range(NST):
            k, pb16 = transposes(xg[:, st, 0:1024], [kxg], 8)
            P.op("act", ACTF(xT[:, :, st * 128:(st + 1) * 128], v3(pb16[:, 0:1024]), AF.Copy), reads=[k], writes=[kxT])
        for fc in range(4):
            fs = slice(fc * 128, (fc + 1) * 128)
            kq, bank = ps()
            fns = [MM(bank[:, 0:CAP], Wg[:, kc, fs], xT[:, kc, :], kc == 0, kc == 7) for kc in range(8)]
            P.op("pe", fns, reads=[kxT, kw + "g"], writes=[kq])
            kq2, bank2 = ps()
            fns = [MM(bank2[:, 0:CAP], Wu[:, kc, fs], xT[:, kc, :], kc == 0, kc == 7) for kc in range(8)]
            P.op("pe", fns, reads=[kxT, kw + "u"], writes=[kq2])
            P.op("act", ACTF(hsl, bank[:, 0:CAP], AF.Silu), reads=[kq], writes=["hsl"])
            P.op("dve", TT(hb[:, fc, :], hsl, bank2[:, 0:CAP], ALU.mult), reads=["hsl", kq2], writes=[khb + "_%d" % fc])
        for st in range(NST):
            y_, ky = yw[ywi % 4], "yw%d" % (ywi % 4)
            ywi += 1
            for dh in range(2):
                cs = slice(dh * 512, (dh + 1) * 512)
                kq, bank = ps()
                P.op("pe", [MM(bank, hb[:, fc, st * 128:(st + 1) * 128], Wd[:, fc, cs], fc == 0, fc == 3) for fc in range(4)],
                     reads=[khb + "_%d" % fc for fc in range(4)] + [kw + "d"], writes=[kq])
                P.op("act", ACTF(y_[:, cs], bank, AF.Copy, scale=xg[:, st, 1024:1026].bitcast(F32)), reads=[kq, kxg], writes=[ky + "_%d" % dh])
            P.dma("pool", lambda e, y_=y_, xg=xg, st=st: e.indirect_dma_start(
                out=yo_d[:, :], out_offset=bass.IndirectOffsetOnAxis(ap=xg[:, st, 1026:1028].bitcast(I32), axis=0), in_=y_[:, :], in_offset=None,
                bounds_check=P.regs["yo"], oob_is_err=False), reads=[ky + "_0", ky + "_1", kxg], writes=[("Yo", e_, st)])
    P.barrier()
    for i in range(NT_OWN):
        yl, kyl = yld[i % 2], "yld%d" % (i % 2)
        P.dma("sp", lambda e, i=i, yl=yl: e.dma_start(out=yl, in_=yo_d[i * 256:(i + 1) * 256, :].rearrange("(p k) n -> p k n", k=2)),
              writes=[kyl])
        P.op("dve", TT(acc[:, i, :], acc[:, i, :], yl[:, 0, :], ALU.add), reads=[kyl, ("acc", i)], writes=[("acc", i)])
        P.op("dve", TT(acc[:, i, :], acc[:, i, :], yl[:, 1, :], ALU.add), reads=[kyl, ("acc", i)], writes=[("acc", i)])
        ln_tile(acc[:, i, :], V2("l2g"), V2("l2b"), ("acc", i), "vec2")
        P.dma("pool", lambda e, i=i: e.dma_start(out=out_d[i * 128:(i + 1) * 128, :], in_=acc[:, i, :]), reads=[("acc", i)], writes=[("out", i)])
    return nc, P, es


def _prep_inputs(inputs):
    f = lambda k: np.asarray(inputs[k], dtype=np.float32)
    x = f("x")
    p = f("p")[0]
    bc = lambda v: np.broadcast_to(np.asarray(v, np.float32).reshape(1, -1), (128, np.asarray(v).size))
    vec1 = np.ascontiguousarray(np.concatenate([
        bc(f("ln_emb_g")), bc(f("ln_emb_b")), bc(f("mu_shift")[0]), bc(f("w0")[0]), bc(f("a0")[0]), bc(f("k_k")[0]),
        bc(f("k_a")[0]), bc(f("r_k")[0].reshape(-1)), bc(f("gn_g")[0]), bc(f("gn_b")[0]), bc(f("gmlp_ln_g")[0]),
        bc(f("gmlp_ln_b")[0])], axis=1))
    brt = np.concatenate([f("b_group_router")[0].reshape(-1), f("b_expert_router")[0].reshape(-1)])
    vec2 = np.ascontiguousarray(np.concatenate([
        bc(f("ln1_g")[0]), bc(f("ln1_b")[0]), bc(f("ln2_g")[0]), bc(f("ln2_b")[0]), bc(f("b_ple_gate")[0]), bc(brt),
        bc(np.arange(NEXP, dtype=np.float32) * CAP), np.arange(128, dtype=np.float32).reshape(128, 1)], axis=1))
    wrouter = np.ascontiguousarray(np.concatenate([f("w_group_router")[0], f("w_expert_router")[0].reshape(D, 32)], axis=1))
    wlora = np.ascontiguousarray(np.concatenate([f("w_decay_up")[0], f("w_iclr_up")[0]], axis=0))
    wsT = np.ascontiguousarray(np.transpose(f("w_spatial")[0], (2, 0, 1)).reshape(128, 512))
    bsp = np.ascontiguousarray(f("b_spatial")[0].T)
    idx = np.arange(128)
    same = (idx[:, None] // 64) == (idx[None, :] // 64)
    ident = np.eye(128, dtype=np.float32)
    m_su = (same & (idx[:, None] < idx[None, :])).astype(np.float32)
    m_iu = (same & (idx[:, None] <= idx[None, :])).astype(np.float32)
    m_sl = (same & (idx[:, None] > idx[None, :])).astype(np.float32)
    tri = m_iu.copy()
    m_gm = (idx[:, None] <= idx[None, :]).astype(np.float32)
    cmats = np.ascontiguousarray(np.concatenate([ident, m_su, m_iu, m_sl, tri, m_gm], axis=1))
    sel = np.zeros((128, 2), np.float32)
    sel[63, 0] = 1.0
    sel[127, 1] = 1.0
    tmpl = np.zeros((128, ROW), dtype=ml_dtypes.bfloat16)
    meta = np.zeros((128, 2), np.int32)
    meta[:, 1] = 1 << 24
    tmpl[:, 1024:1028] = meta.view(ml_dtypes.bfloat16).reshape(128, 4)
    shared = {
        "tmpl": tmpl,
        "w_in": np.ascontiguousarray(f("w_in")[0]), "w_out": np.ascontiguousarray(f("w_out")[0]),
        "wlora": wlora, "wgate": np.ascontiguousarray(f("w_gate_up")[0]), "wrouter": wrouter,
        "w_exp_gate": np.ascontiguousarray(f("w_exp_gate")[0]), "w_exp_up": np.ascontiguousarray(f("w_exp_up")[0]),
        "w_exp_down": np.ascontiguousarray(f("w_exp_down")[0]), "w_ple_gate": np.ascontiguousarray(f("w_ple_gate")[0]),
        "w_ple_proj": np.ascontiguousarray(f("w_ple_proj")[0]), "wsT": wsT, "bsp": bsp, "vec1": vec1, "vec2": vec2,
        "cmats": cmats, "sel": sel,
    }
    in_maps = []
    for c in range(8):
        b, hf = c // 2, c % 2
        xs = np.zeros((SEQ, D), np.float32)
        if hf == 1:
            xs[:] = x[b]
        else:
            xs[HALF:] = x[b, :HALF]
        m = dict(shared)
        m["xs"] = xs
        m["p_s"] = np.ascontiguousarray(p[b, hf * HALF:(hf + 1) * HALF])
        m["flag"] = np.full((128, 1), float(hf), np.float32)
        in_maps.append(m)
    return in_maps


_CACHE = {}


def kernel(**inputs):
    in_maps = _prep_inputs(inputs)
    if "nc" not in _CACHE:
        nc, P, es = build()
        with nc.Block() as block:
            P.replay(block)
        es.close()
        _CACHE["nc"] = nc
    nc = _CACHE["nc"]
    res = run_bass_kernel_spmd(nc, in_maps, core_ids=list(range(8)))
    out = np.zeros((NB, SEQ, D), np.float32)
    for c in range(8):
        b, hf = c // 2, c % 2
        out[b, hf * HALF:(hf + 1) * HALF] = res.results[c]["out"]
    return out
```

```python
import math
from contextlib import ExitStack
import numpy as np
import ml_dtypes
import concourse.bass as bass
import concourse.mybir as mybir
from concourse.bass_utils import run_bass_kernel_spmd

F32 = mybir.dt.float32
BF16 = mybir.dt.bfloat16
AF = mybir.ActivationFunctionType
ALU = mybir.AluOpType
AX = mybir.AxisListType

D = 1024
SEQ = 4096
NB = 4
HALF = 2048
NT_OWN = 16
NH = 8
DS = math.exp(-0.5)
ALPHA = 2.0 ** 0.25
LN_EPS = 1e-5
GN_EPS = 64e-5
NEXP = 32
DEXP = 512
PIPELINE = True
L2_DVE = False
DBG_STOP = None
SIM_MODE = False
N_TILES = 32
OWN_FROM = 16
CAP = 256
ROW = 1028
I32 = mybir.dt.int32


class Prog:
    CE = ("pe", "act", "dve", "pool")

    def __init__(self, nc, es, n_dma_sems=24):
        self.nc = nc
        self.q = {e: [] for e in ("pe", "act", "dve", "pool", "sp")}
        self.cnt = {e: 0 for e in self.CE}
        self.lastw = {}
        self.readers = {}
        self.csem = {e: es.enter_context(nc.semaphore("cs_" + e)) for e in self.CE}
        self.dsem = [es.enter_context(nc.semaphore("ds%d" % i)) for i in range(n_dma_sems)]
        self.dcnt = [0] * n_dma_sems
        self.n_rr = n_dma_sems
        self.es = es
        self.drr = 0
        self.all_dma = []
        self.bar = set()
        self.reg_req = {}
        self.regs = {}

    def barrier(self):
        b = set()
        for e in self.CE:
            if self.cnt[e] > 0:
                b.add(("c", e, self.cnt[e]))
        for i, c in enumerate(self.dcnt):
            if c > 0:
                b.add(("d", i, c))
        self.bar = b

    def _deps(self, reads, writes):
        deps = set()
        for b in reads:
            if b in self.lastw:
                deps.add(self.lastw[b])
        for b in writes:
            if b in self.lastw:
                deps.add(self.lastw[b])
            deps.update(self.readers.get(b, ()))
        deps |= self.bar
        return deps

    def _commit(self, tok, reads, writes):
        for b in reads:
            self.readers.setdefault(b, []).append(tok)
        for b in writes:
            self.lastw[b] = tok
            self.readers[b] = []

    def op(self, eng, fns, reads=(), writes=()):
        if not isinstance(fns, (list, tuple)):
            fns = [fns]
        deps = self._deps(reads, writes)
        self.cnt[eng] += 1
        tok = ("c", eng, self.cnt[eng])
        self.q[eng].append(("op", list(fns), deps, tok))
        self._commit(tok, reads, writes)
        return tok

    def dma(self, eng, fn, reads=(), writes=()):
        deps = self._deps(reads, writes)
        if eng == "pool" and SIM_MODE:
            self.dsem.append(self.es.enter_context(self.nc.semaphore("dp%d" % len(self.dsem))))
            self.dcnt.append(0)
            s = len(self.dsem) - 1
        else:
            s = self.drr
            self.drr = (self.drr + 1) % self.n_rr
        prev = self.dcnt[s]
        self.dcnt[s] += 16
        tok = ("d", s, self.dcnt[s])
        self.q[eng].append(("dma", fn, deps, tok, prev))
        self._commit(tok, reads, writes)
        self.all_dma.append(tok)
        return tok

    def replay(self, block, final_eng="sp"):
        prog = self

        def run(ename, e):
            waited = {}
            if ename == "pool":
                for nm, val in prog.reg_req.items():
                    prog.regs[nm] = e.to_reg(val)

            def wait(tok):
                if tok[0] == "c":
                    if tok[1] == ename and ename == "pe":
                        return
                    key = ("c", tok[1])
                    sem = prog.csem[tok[1]]
                else:
                    key = ("d", tok[1])
                    sem = prog.dsem[tok[1]]
                if waited.get(key, 0) >= tok[2]:
                    return
                waited[key] = tok[2]
                e.wait_ge(sem, tok[2])

            for item in prog.q[ename]:
                if item[0] == "op":
                    _, fns, deps, tok = item
                    for d in sorted(deps):
                        wait(d)
                    for f in fns[:-1]:
                        f(e)
                    fns[-1](e).then_inc(prog.csem[ename], 1)
                else:
                    _, fn, deps, tok, prev = item
                    for d in sorted(deps):
                        wait(d)
                    if prev > 0:
                        wait(("d", tok[1], prev))
                    fn(e).then_inc(prog.dsem[tok[1]], 16)
            if ename == final_eng:
                for s in range(len(prog.dsem)):
                    if prog.dcnt[s] > 0:
                        wait(("d", s, prog.dcnt[s]))
                for ce in prog.CE:
                    if prog.cnt[ce] > 0:
                        wait(("c", ce, prog.cnt[ce]))

        block.tensor(lambda e: run("pe", e))
        block.scalar(lambda e: run("act", e))
        block.vector(lambda e: run("dve", e))
        block.gpsimd(lambda e: run("pool", e))
        block.sync(lambda e: run("sp", e))


def MM(out, l, r, st=True, sp=True):
    return lambda e: e.matmul(out, l, r, start=st, stop=sp)


def TR(out, in_, ident):
    return lambda e: e.transpose(out, in_, ident)


def ACTF(out, in_, func, **kw):
    return lambda e: e.activation(out=out, in_=in_, func=func, **kw)


def TT(out, a, b, op):
    return lambda e: e.tensor_tensor(out=out, in0=a, in1=b, op=op)


def TS(out, a, s1, s2=None, op0=ALU.mult, op1=None):
    if op1 is None:
        return lambda e: e.tensor_scalar(out=out, in0=a, scalar1=s1, scalar2=None, op0=op0)
    return lambda e: e.tensor_scalar(out=out, in0=a, scalar1=s1, scalar2=s2, op0=op0, op1=op1)


def STT(out, in0, scalar, in1, op0, op1):
    return lambda e: e.scalar_tensor_tensor(out=out, in0=in0, scalar=scalar, in1=in1, op0=op0, op1=op1)


def CP(out, in_):
    return lambda e: e.tensor_copy(out, in_)


def RED(out, in_, op=ALU.add):
    return lambda e: e.tensor_reduce(out=out, in_=in_, axis=AX.X, op=op)


class Arena:
    def __init__(self, big, nbytes):
        self.big = big
        self.n = nbytes
        self.off = 0
        self.mark_ = 0

    def alloc(self, shape, dt):
        esz = 2 if dt == BF16 else 4
        n = 1
        for d in shape[1:]:
            n *= d
        nb = n * esz
        self.off = (self.off + 63) // 64 * 64
        o = self.off
        self.off += nb
        assert self.off <= self.n, ("SBUF arena overflow", self.off, self.n)
        v = self.big[0:shape[0], o // 2:(o + nb) // 2]
        if dt != BF16:
            v = v.bitcast(dt)
        if len(shape) == 3:
            v = v.rearrange("p (a b) -> p a b", a=shape[1])
        elif len(shape) == 4:
            v = v.rearrange("p (a b c) -> p a b c", a=shape[1], b=shape[2])
        return v


VEC1 = (("lneg", D), ("lneb", D), ("mu", 1792), ("w0", 512), ("a0", 512), ("kk", 512), ("ka", 512),
        ("rk", 512), ("gng", 512), ("gnb", 512), ("glg", 512), ("glb", 512))
VEC2 = (("l1g", D), ("l1b", D), ("l2g", D), ("l2b", D), ("bpg", D), ("brt", 36), ("eC", 32), ("pid", 1))


def _offs(spec):
    d = {}
    o = 0
    for nm, n in spec:
        d[nm] = (o, n)
        o += n
    return d, o


def build(debug=None):
    nc = bass.Bass("TRN2", target_bir_lowering=False)
    es = ExitStack()

    def din(name, shape, dt=F32):
        return nc.dram_tensor(name, list(shape), dt, kind="ExternalInput").ap()

    xs = din("xs", [SEQ, D])
    pin = din("p_s", [HALF, 256])
    flag_d = din("flag", [128, 1])
    w_in = din("w_in", [D, 2816])
    w_out = din("w_out", [D, D])
    wlora_d = din("wlora", [128, 512])
    wgate_d = din("wgate", [128, 512])
    wrt_d = din("wrouter", [D, 36])
    w_eg = din("w_exp_gate", [NEXP, D, DEXP])
    w_eu = din("w_exp_up", [NEXP, D, DEXP])
    w_ed = din("w_exp_down", [NEXP, DEXP, D])
    w_pg = din("w_ple_gate", [D, D])
    w_pp = din("w_ple_proj", [256, D])
    wsT_d = din("wsT", [128, 512])
    bsp_d = din("bsp", [128, 4])
    O1, NV1 = _offs(VEC1)
    O2, NV2 = _offs(VEC2)
    vec1_d = din("vec1", [128, NV1])
    vec2_d = din("vec2", [128, NV2])
    cm_d = din("cmats", [128, 6 * 128])
    sel_d = din("sel", [128, 2])
    tmpl_d = din("tmpl", [128, ROW], BF16)

    out_d = nc.dram_tensor("out", [HALF, D], F32, kind="ExternalOutput").ap()
    xn_scr = nc.dram_tensor("xn_scr", [HALF, D], F32, kind="Internal").ap()
    ym_scr = nc.dram_tensor("ym_scr", [HALF, D], BF16, kind="Internal").ap()
    xg_d = nc.dram_tensor("xg_scr", [NEXP * CAP, ROW], BF16, kind="Internal").ap()
    yo_d = nc.dram_tensor("yo_scr", [2 * HALF, D], F32, kind="Internal").ap()
    wout_b = nc.dram_tensor("wout_b", [D, D], BF16, kind="Internal").ap()
    wpg_b = nc.dram_tensor("wpg_b", [D, D], BF16, kind="Internal").ap()
    wpp_b = nc.dram_tensor("wpp_b", [256, D], BF16, kind="Internal").ap()
    weg_b = nc.dram_tensor("weg_b", [NEXP, D, DEXP], BF16, kind="Internal").ap()
    weu_b = nc.dram_tensor("weu_b", [NEXP, D, DEXP], BF16, kind="Internal").ap()
    wed_b = nc.dram_tensor("wed_b", [NEXP, DEXP, D], BF16, kind="Internal").ap()
    dbg_d = None
    if debug:
        dbg_d = nc.dram_tensor("dbg", [HALF, D], F32, kind="ExternalOutput").ap()

    NBYTES = 212480
    big = es.enter_context(nc.sbuf_tensor("big", [128, NBYTES // 2], BF16))
    psum = es.enter_context(nc.psum_tensor("psum", [128, 8, 512], F32))
    P = Prog(nc, es)
    P.reg_req = {"xg": NEXP * CAP - 1, "yo": 2 * HALF - 1}
    ps_rr = [0, 0, 0]
    ps_stream = [0]

    def ps():
        st = ps_stream[0]
        if st == 0:
            b = ps_rr[0]
            ps_rr[0] = (b + 1) % 8
        elif st == 1:
            b = ps_rr[1]
            ps_rr[1] = (b + 1) % 4
        else:
            b = 4 + ps_rr[2]
            ps_rr[2] = (ps_rr[2] + 1) % 4
        return ("ps", b), psum[:, b, :]

    A = Arena(big, NBYTES)
    cm32 = A.alloc([128, 6 * 128], F32)
    identb = A.alloc([128, 128], BF16)
    sel = A.alloc([128, 2], F32)
    flag = A.alloc([128, 1], F32)
    eps_t = A.alloc([128, 2], F32)
    st12 = A.alloc([128, 12], F32)
    mv = A.alloc([128, 2], F32)
    rstd = A.alloc([128, 1], F32)
    s8 = A.alloc([128, 48], F32)
    phase_mark = A.off

    ident = identb
    ident32 = cm32[:, 0:128]
    m_su, m_iu, m_sl = cm32[:, 128:256], cm32[:, 256:384], cm32[:, 384:512]
    tri32 = cm32[:, 512:640]
    m_gm = cm32[:, 640:768]

    P.dma("sp", lambda e: e.dma_start(out=cm32, in_=cm_d), writes=["cm32"])
    P.dma("sp", lambda e: e.dma_start(out=sel, in_=sel_d), writes=["sel"])
    P.dma("sp", lambda e: e.dma_start(out=flag, in_=flag_d), writes=["flag"])
    P.dma("pool", lambda e: e.dma_start(out=identb, in_=cm_d[:, 0:128]), writes=["cmb"])
    P.op("pool", lambda e: e.memset(eps_t[:, 0:1], LN_EPS), writes=["eps"])
    for t in range(NEXP * CAP // 128):
        P.dma("sp", lambda e, t=t: e.dma_start(out=xg_d[t * 128:(t + 1) * 128, :], in_=tmpl_d), writes=["XgT%d" % t])
    P.op("pool", lambda e: e.memset(eps_t[:, 1:2], GN_EPS), writes=["eps"])

    def ln_tile(x, gk, bk, key, vkey):
        P.op("dve", lambda e: e.bn_stats(st12[:, 0:6], x[:, 0:512]), reads=[key], writes=["st12a"])
        P.op("dve", lambda e: e.bn_stats(st12[:, 6:12], x[:, 512:1024]), reads=[key], writes=["st12b"])
        P.op("dve", lambda e: e.bn_aggr(mv, st12), reads=["st12a", "st12b"], writes=["mv"])
        P.op("act", ACTF(rstd, mv[:, 1:2], AF.Sqrt, bias=eps_t[:, 0:1]), reads=["mv", "eps"], writes=["rstd"])
        P.op("dve", lambda e: e.reciprocal(rstd, rstd), reads=["rstd"], writes=["rstd"])
        P.op("dve", TS(x, x, mv[:, 0:1], rstd[:, 0:1], ALU.subtract, ALU.mult), reads=[key, "mv", "rstd"], writes=[key])
        P.op("dve", TT(x, x, gk, ALU.mult), reads=[key, vkey], writes=[key])
        P.op("dve", TT(x, x, bk, ALU.add), reads=[key, vkey], writes=[key])

    def v3(t, h=8):
        return t.rearrange("p (h j) -> p h j", h=h)

    def hv(t, h):
        return t[:, h * 64:(h + 1) * 64]

    def transposes(src, src_keys, n, width=128):
        k, bank = ps()
        pb16 = bank.bitcast(BF16)
        fns = [TR(pb16[0:width, j * 128:(j + 1) * 128], src[:, j * width:(j + 1) * width], ident) for j in range(n)]
        P.op("pe", fns, reads=list(src_keys) + ["cmb"], writes=[k])
        return k, pb16

    Win = A.alloc([128, 8, 2816], BF16)
    wlora = A.alloc([128, 512], BF16)
    wgate = A.alloc([128, 512], BF16)
    wsT = A.alloc([128, 512], BF16)
    bsp = A.alloc([128, 4], F32)
    vec1 = A.alloc([128, NV1], F32)

    def V1(nm):
        o, n = O1[nm]
        return vec1[:, o:o + n]

    def dbl(shape, dt):
        return [A.alloc(shape, dt) for _ in range(2)]

    xt = A.alloc([128, D], F32)
    xnb = A.alloc([128, D], BF16)
    xnT = [A.alloc([128, 8, 129], BF16) for _ in range(2)]
    h32 = A.alloc([128, 1792], F32)
    lor = A.alloc([128, 256], BF16)
    lorT = A.alloc([128, 256], BF16)
    sg = A.alloc([128, 512], F32)
    eW = A.alloc([128, 512], F32)
    eWi = A.alloc([128, 512], F32)
    eWm = A.alloc([128, 512], F32)
    al = A.alloc([128, 512], F32)
    kkn = sg
    kp = A.alloc([128, 512], F32)
    tmp = A.alloc([128, 512], F32)
    tmpB = A.alloc([128, 512], F32)
    g32_ = dbl([128, 512], F32)
    zvn = A.alloc([128, 512], BF16)
    AZ_ = dbl([128, 8, 128], BF16)
    Bt_ = dbl([128, 512], BF16)
    Kt_ = dbl([128, 512], BF16)
    Rt_ = dbl([128, 512], BF16)
    Vb_ = dbl([128, 512], BF16)
    ART_ = dbl([64, 8, 256], BF16)
    BTc_ = dbl([64, 8, 128], BF16)
    KTc_ = dbl([64, 8, 128], BF16)
    Nm_ = dbl([128, 8, 128], BF16)
    NmT_ = dbl([128, 8, 128], BF16)
    BRm_ = dbl([128, 8, 128], BF16)
    KRm_ = dbl([128, 8, 128], BF16)
    KAm_ = dbl([128, 8, 128], BF16)
    Wc_ = dbl([64, 8, 2], F32)
    rkf_ = dbl([128, 8], F32)
    PT3 = A.alloc([128, 8, 384], BF16)
    PTb1 = A.alloc([128, 8, 128], BF16)
    TTm = PT3[:, :, 128:256]
    AV = A.alloc([128, 8, 128], BF16)
    AhT = A.alloc([64, 8, 128], BF16)
    PpT = A.alloc([64, 2, 8, 64], BF16)
    Qp = A.alloc([64, 2, 512], F32)
    N32 = [A.alloc([64, 512], F32) for _ in range(2)]
    N16 = [A.alloc([64, 512], BF16) for _ in range(4)]
    t1 = tmpB[0:64, :]
    Usb = A.alloc([128, 512], BF16)
    yA = A.alloc([128, 512], BF16)
    yB_ = dbl([128, 512], BF16)
    y32 = A.alloc([128, 512], F32)
    zu = A.alloc([128, 512], F32)
    zv = A.alloc([128, 512], F32)
    ZU, ZV, Y32 = "zu", "zv", "y32"
    print("phase1 sbuf bytes", A.off)

    P.dma("sp", lambda e: e.dma_start(out=vec1, in_=vec1_d), writes=["vec1"])
    P.dma("sp", lambda e: e.dma_start(out=bsp, in_=bsp_d), writes=["bsp"])
    P.dma("sp", lambda e: e.dma_start(out=tmp, in_=wsT_d), writes=["tmp"])
    P.dma("pool", lambda e: e.dma_start(out=wlora, in_=wlora_d), writes=["wlora"])
    P.dma("pool", lambda e: e.dma_start(out=wgate, in_=wgate_d), writes=["wgate"])
    for kc in range(8):
        P.dma("pool", lambda e, kc=kc: e.dma_start(out=Win[:, kc, :], in_=w_in[kc * 128:(kc + 1) * 128, :]), writes=["Win"])
    P.op("dve", TT(v3(wsT, 4), v3(tmp, 4), m_gm.unsqueeze(1).to_broadcast([128, 4, 128]), ALU.mult),
         reads=["tmp", "cm32"], writes=["wsT"])
    P.dma("pool", lambda e: e.dma_start(out=wout_b, in_=w_out), writes=["woutb"])
    P.dma("pool", lambda e: e.dma_start(out=wpg_b, in_=w_pg), writes=["wpgb"])
    P.dma("pool", lambda e: e.dma_start(out=wpp_b, in_=w_pp), writes=["wppb"])
    P.op("pool", lambda e: e.memset(N32[0], 0.0), writes=["N32_0"])
    P.op("pool", lambda e: e.memset(N16[0], 0.0), writes=["N16_0"])
    P.op("pool", lambda e: e.memset(xnT[1][:, :, 128:129], 0.0), writes=["xnT1"])

    su_b = m_su.unsqueeze(1).to_broadcast([128, 4, 128])
    iu_b = m_iu.unsqueeze(1).to_broadcast([128, 4, 128])
    sl_b = m_sl.unsqueeze(1).to_broadcast([128, 4, 128])
    state = {"n": 0}
    a_done = [False] * 33
    j_done = [False] * 33
    n_tiles = N_TILES

    def tile_gen(i):
        own = i >= OWN_FROM
        par = i % 2
        K = lambda nm: "%s_%d" % (nm, par)
        g32, AZ, Bt, Kt, Rt, Vb = g32_[par], AZ_[par], Bt_[par], Kt_[par], Rt_[par], Vb_[par]
        ART, BTc, KTc = ART_[par], BTc_[par], KTc_[par]
        yB = yB_[par]
        Nm, NmT, BRm, KRm, KAm, Wc, rkf = Nm_[par], NmT_[par], BRm_[par], KRm_[par], KAm_[par], Wc_[par], rkf_[par]
        cur, prv = xnT[i % 2], xnT[(i + 1) % 2]
        kcur, kprv = "xnT%d" % (i % 2), "xnT%d" % ((i + 1) % 2)
        e_ = i
        P.dma("pool", lambda e: e.dma_start(out=weg_b[e_], in_=w_eg[e_]), writes=[("wegb", e_)])
        P.dma("pool", lambda e: e.dma_start(out=weu_b[e_], in_=w_eu[e_]), writes=[("weub", e_)])
        P.dma("pool", lambda e: e.dma_start(out=wed_b[e_], in_=w_ed[e_]), writes=[("wedb", e_)])
        while i > 0 and not a_done[i - 1]:
            yield
        P.dma("sp", lambda e: e.dma_start(out=xt, in_=xs[i * 128:(i + 1) * 128, :]), writes=["xt"])
        ln_tile(xt, V1("lneg"), V1("lneb"), "xt", "vec1")
        P.op("act", ACTF(xnb, xt, AF.Copy), reads=["xt"], writes=["xnb"])
        if own:
            P.dma("pool", lambda e: e.dma_start(out=xn_scr[(i - OWN_FROM) * 128:(i - OWN_FROM + 1) * 128, :], in_=xt), reads=["xt"], writes=["xnscr"])
        yield
        k, pb16 = transposes(xnb, ["xnb"], 8)
        P.op("act", ACTF(cur[:, :, 1:129], v3(pb16[:, 0:1024]), AF.Copy), reads=[k], writes=[kcur])
        if i == OWN_FROM:
            P.op("act", ACTF(cur[:, :, 0:1], prv[:, :, 128:129], AF.Copy, scale=flag[:, 0:1]), reads=[kprv, "flag"], writes=[kcur])
        else:
            P.op("act", ACTF(cur[:, :, 0:1], prv[:, :, 128:129], AF.Copy), reads=[kprv], writes=[kcur])
        a_done[i] = True
        yield
        for (c0, cn) in ((0, 512), (512, 512), (1024, 512), (1536, 256)):
            if c0 == 0 and not own:
                continue
            k1, b1 = ps()
            k2, b2 = ps()
            P.op("pe", [MM(b1[:, 0:cn], cur[:, kc, 1:129], Win[:, kc, c0:c0 + cn], kc == 0, kc == 7) for kc in range(8)],
                 reads=[kcur, "Win"], writes=[k1])
            P.op("pe", [MM(b2[:, 0:cn], cur[:, kc, 0:128], Win[:, kc, c0:c0 + cn], kc == 0, kc == 7) for kc in range(8)],
                 reads=[kcur, "Win"], writes=[k2])
            hk = "h32_%d" % c0
            hs_ = h32[:, c0:c0 + cn]
            P.op("act", ACTF(hs_, b1[:, 0:cn], AF.Copy), reads=[k1], writes=[hk])
            P.op("dve", TT(tmp[:, 0:cn], b2[:, 0:cn], hs_, ALU.subtract), reads=[k2, hk], writes=["tmp"])
            P.op("dve", TT(tmp[:, 0:cn], tmp[:, 0:cn], V1("mu")[:, c0:c0 + cn], ALU.mult), reads=["tmp", "vec1"], writes=["tmp"])
            P.op("dve", TT(hs_, hs_, tmp[:, 0:cn], ALU.add), reads=["tmp", hk], writes=[hk])
            if not own:
                P.op("dve", TS(hs_, hs_, flag[:, 0:1]), reads=[hk, "flag"], writes=[hk])
            yield
        if own:
            for gi, dst, dk in ((0, zu, ZU), (1, zv, ZV)):
                c0 = 1792 + gi * 512
                kq, bank = ps()
                P.op("pe", [MM(bank, cur[:, kc, 1:129], Win[:, kc, c0:c0 + 512], kc == 0, kc == 7) for kc in range(8)], reads=[kcur, "Win"], writes=[kq])
                P.op("act", ACTF(dst, bank, AF.Gelu), reads=[kq], writes=[dk])
                yield
            zv4 = v3(zv, 4)
            P.op("dve", RED(s8[:, 32:36], zv4), reads=[ZV], writes=["s8e"])
            P.op("dve", TT(tmp, zv, zv, ALU.mult), reads=[ZV], writes=["tmp"])
            P.op("dve", RED(s8[:, 36:40], v3(tmp, 4)), reads=["tmp"], writes=["s8f"])
            P.op("dve", TS(s8[:, 32:36], s8[:, 32:36], 1.0 / 128), reads=["s8e"], writes=["s8e"])
            P.op("dve", TT(s8[:, 40:44], s8[:, 32:36], s8[:, 32:36], ALU.mult), reads=["s8e"], writes=["s8g"])
            P.op("dve", STT(s8[:, 36:40], s8[:, 36:40], 1.0 / 128, s8[:, 40:44], ALU.mult, ALU.subtract), reads=["s8f", "s8g"], writes=["s8f"])
            P.op("act", ACTF(s8[:, 36:40], s8[:, 36:40], AF.Sqrt, bias=eps_t[:, 0:1]), reads=["s8f", "eps"], writes=["s8f"])
            P.op("dve", lambda e: e.reciprocal(s8[:, 36:40], s8[:, 36:40]), reads=["s8f"], writes=["s8f"])
            yield
            P.op("dve", TT(zv4, zv4, s8[:, 32:36].unsqueeze(2).to_broadcast([128, 4, 128]), ALU.subtract), reads=[ZV, "s8e"], writes=[ZV])
            P.op("dve", TT(zv4, zv4, s8[:, 36:40].unsqueeze(2).to_broadcast([128, 4, 128]), ALU.mult), reads=[ZV, "s8f"], writes=[ZV])
            P.op("dve", TT(zv, zv, V1("glg"), ALU.mult), reads=[ZV, "vec1"], writes=[ZV])
            P.op("dve", TT(zvn, zv, V1("glb"), ALU.add), reads=[ZV, "vec1"], writes=["zvn"])
            yield
            kq, bank = ps()
            P.op("pe", [MM(bank[:, g * 128:(g + 1) * 128], wsT[:, g * 128:(g + 1) * 128], zvn[:, g * 128:(g + 1) * 128]) for g in range(4)],
                 reads=["wsT", "zvn"], writes=[kq])
            for g in range(4):
                P.op("dve", STT(yB[:, g * 128:(g + 1) * 128], bank[:, g * 128:(g + 1) * 128], bsp[:, g:g + 1],
                                zu[:, g * 128:(g + 1) * 128], ALU.add, ALU.mult), reads=[kq, "bsp", ZU], writes=[K("yB%d" % g)])
            P.dma("pool", lambda e: e.dma_start(out=ym_scr[(i - OWN_FROM) * 128:(i - OWN_FROM + 1) * 128, 512:1024], in_=yB),
                  reads=[K("yB%d" % g) for g in range(4)], writes=["ymscrB"])
            yield
        r_, k_, v_ = h32[:, 0:512], h32[:, 512:1024], h32[:, 1024:1536]
        P.op("act", ACTF(lor[:, 0:64], h32[:, 1536:1600], AF.Tanh), reads=["h32_1536"], writes=["lor"])
        P.op("act", ACTF(lor[:, 64:128], h32[:, 1600:1664], AF.Copy), reads=["h32_1536"], writes=["lor"])
        P.op("act", ACTF(lor[:, 128:256], h32[:, 1664:1792], AF.Sigmoid), reads=["h32_1536"], writes=["lor"])
        k, pb16 = transposes(lor, ["lor"], 2)
        P.op("act", ACTF(lorT, pb16[:, 0:256], AF.Copy), reads=[k], writes=["lorT"])
        yield
        kd, bd = ps()
        P.op("pe", MM(bd, lorT[0:64, 0:128], wlora[0:64, :]), reads=["lorT", "wlora"], writes=[kd])
        P.op("dve", TT(sg, bd, V1("w0"), ALU.add), reads=[kd, "vec1"], writes=["sg"])
        P.op("act", ACTF(sg, sg, AF.Sigmoid), reads=["sg"], writes=["sg"])
        ka_, ba = ps()
        P.op("pe", MM(ba, lorT[64:128, 0:128], wlora[64:128, :]), reads=["lorT", "wlora"], writes=[ka_])
        P.op("dve", TT(al, ba, V1("a0"), ALU.add), reads=[ka_, "vec1"], writes=["al"])
        P.op("act", ACTF(al, al, AF.Sigmoid), reads=["al"], writes=["al"])
        if own:
            kg, bg = ps()
            P.op("pe", MM(bg, lorT[:, 128:256], wgate), reads=["lorT", "wgate"], writes=[kg])
            P.op("act", ACTF(g32, bg, AF.Copy), reads=[kg], writes=[K("g32")])
        yield
        kc_, bc = ps()
        P.op("pe", MM(bc, tri32, sg), reads=["sg", "cm32"], writes=[kc_])
        P.op("act", ACTF(eW, bc, AF.Exp, scale=-DS), reads=[kc_], writes=["eW"])
        P.op("act", ACTF(eWi, bc, AF.Exp, scale=DS), reads=[kc_], writes=["eWi"])
        P.op("dve", TT(tmp, bc, sg, ALU.subtract), reads=[kc_, "sg", "eW", "eWi"], writes=["tmp"])
        P.op("act", ACTF(eWm, tmp, AF.Exp, scale=-DS), reads=["tmp"], writes=["eWm"])
        yield
        P.op("dve", TT(kkn, k_, V1("kk"), ALU.mult), reads=["h32_512", "vec1"], writes=["sg"])
        P.op("dve", TT(tmp, kkn, kkn, ALU.mult), reads=["sg"], writes=["tmp"])
        P.op("dve", RED(s8[:, 0:8], v3(tmp)), reads=["tmp"], writes=["s8a"])
        P.op("dve", TS(s8[:, 0:8], s8[:, 0:8], 1e-24, None, ALU.max), reads=["s8a"], writes=["s8a"])
        P.op("act", ACTF(s8[:, 0:8], s8[:, 0:8], AF.Sqrt), reads=["s8a"], writes=["s8a"])
        P.op("dve", lambda e: e.reciprocal(s8[:, 0:8], s8[:, 0:8]), reads=["s8a"], writes=["s8a"])
        P.op("dve", TT(v3(kkn), v3(kkn), s8[:, 0:8].unsqueeze(2).to_broadcast([128, 8, 64]), ALU.mult), reads=["sg", "s8a"], writes=["sg"])
        yield
        P.op("dve", STT(kp, al, -1.0, V1("ka"), ALU.add, ALU.mult), reads=["al", "vec1"], writes=["kp"])
        P.op("dve", STT(kp, kp, 1.0, k_, ALU.add, ALU.mult), reads=["kp", "h32_512"], writes=["kp"])
        P.op("dve", STT(AZ[:, :, 0:64], v3(kkn), -1.0, v3(eWm), ALU.mult, ALU.mult), reads=["sg", "eWm"], writes=[K("AZa")])
        P.op("dve", TT(tmp, kkn, al, ALU.mult), reads=["sg", "al"], writes=["tmp"])
        P.op("dve", TT(Bt, tmp, eWi, ALU.mult), reads=["tmp", "eWi"], writes=[K("Bt")])
        P.op("dve", TT(Kt, kp, eWi, ALU.mult), reads=["kp", "eWi"], writes=[K("Kt")])
        P.op("act", ACTF(Vb, v_, AF.Copy), reads=["h32_1024"], writes=[K("Vb")])
        yield
        if own:
            P.op("dve", TT(Rt, r_, eW, ALU.mult), reads=["h32_0", "eW"], writes=[K("Rt")])
            P.op("dve", TT(tmp, r_, kp, ALU.mult), reads=["h32_0", "kp"], writes=["tmp"])
            P.op("dve", TT(tmp, tmp, V1("rk"), ALU.mult), reads=["tmp", "vec1"], writes=["tmp"])
            P.op("dve", RED(rkf, v3(tmp)), reads=["tmp"], writes=[K("rkf")])
        kq, bank = ps()
        P.op("pe", [MM(bank[0:64, h * 2:h * 2 + 2], hv(eW, h), sel) for h in range(8)], reads=["eW", "sel"], writes=[kq])
        P.op("act", ACTF(Wc, v3(bank[0:64, 0:16]), AF.Copy), reads=[kq], writes=[K("Wc")])
        yield
        def tr_heads(srcf, skey, dst, dkey):
            kq, bank = ps()
            pq = bank.bitcast(BF16)
            P.op("pe", [TR(pq[0:64, h * 128:(h + 1) * 128], srcf(h), ident) for h in range(8)], reads=[skey, "cmb"], writes=[kq])
            P.op("act", ACTF(dst, v3(pq[0:64, :]), AF.Copy), reads=[kq], writes=[dkey])
        tr_heads(lambda h: AZ[:, h, 0:64], K("AZa"), ART[:, :, 0:128], K("ARTa"))
        yield
        if own:
            tr_heads(lambda h: hv(Rt, h), K("Rt"), ART[:, :, 128:256], K("ARTr"))
            yield
        tr_heads(lambda h: hv(Bt, h), K("Bt"), BTc, K("BTc"))
        yield
        tr_heads(lambda h: hv(Kt, h), K("Kt"), KTc, K("KTc"))
        yield
        def lmat(lf, rf, rkeys, mask_b, dst, dkey):
            for hg in range(2):
                kq, bank = ps()
                b3 = v3(bank, 4)
                P.op("pe", [MM(b3[:, h - hg * 4, :], lf(h), rf(h)) for h in range(hg * 4, hg * 4 + 4)], reads=rkeys, writes=[kq])
                P.op("dve", TT(dst[:, hg * 4:hg * 4 + 4, :], b3, mask_b, ALU.mult), reads=[kq, "cm32"], writes=[dkey])
        lmat(lambda h: BTc[:, h, :], lambda h: ART[:, h, 0:128], [K("BTc"), K("ARTa")], su_b, Nm, K("Nm"))
        yield
        lmat(lambda h: ART[:, h, 0:128], lambda h: BTc[:, h, :], [K("BTc"), K("ARTa")], sl_b, NmT, K("NmT"))
        yield
        lmat(lambda h: KTc[:, h, :], lambda h: ART[:, h, 0:128], [K("KTc"), K("ARTa")], su_b, KAm, K("KAm"))
        yield
        if own:
            lmat(lambda h: BTc[:, h, :], lambda h: ART[:, h, 128:256], [K("BTc"), K("ARTr")], iu_b, BRm, K("BRm"))
            yield
            lmat(lambda h: KTc[:, h, :], lambda h: ART[:, h, 128:256], [K("KTc"), K("ARTr")], iu_b, KRm, K("KRm"))
            yield
        yield "HALF"
        if DBG_STOP == "E":
            return
        SA, ST, SB = slice(0, 128), slice(128, 256), slice(256, 384)
        P.op("dve", TT(PT3[:, 0:4, ST], Nm[:, 0:4, :], ident32.unsqueeze(1).to_broadcast([128, 4, 128]), ALU.add), reads=[K("Nm"), "cm32"], writes=["T0"])
        P.op("dve", TT(PT3[:, 4:8, ST], Nm[:, 4:8, :], ident32.unsqueeze(1).to_broadcast([128, 4, 128]), ALU.add), reads=[K("Nm"), "cm32"], writes=["T1"])
        for hg in range(2):
            hs = range(hg * 4, hg * 4 + 4)
            kq, bank = ps()
            b3 = v3(bank, 4)
            P.op("pe", [MM(b3[:, h - hg * 4, :], NmT[:, h, :], Nm[:, h, :]) for h in hs], reads=[K("Nm"), K("NmT")], writes=[kq])
            P.op("act", ACTF(PT3[:, hg * 4:hg * 4 + 4, SB], b3, AF.Copy), reads=[kq], writes=["PB%d" % hg])
            kq, bank = ps()
            b3 = v3(bank, 4)
            P.op("pe", [MM(b3[:, h - hg * 4, :], Nm[:, h, :], NmT[:, h, :]) for h in hs], reads=[K("Nm"), K("NmT")], writes=[kq])
            P.op("act", ACTF(PTb1[:, hg * 4:hg * 4 + 4, :], b3, AF.Copy), reads=[kq], writes=["PTb1_%d" % hg])
            yield
        if DBG_STOP == "F1":
            return
        p_in_b = True
        pt_in_1 = True
        for lev in range(2, 6):
            if DBG_STOP == "F%d" % lev:
                return
            for hg in range(2):
                hs = range(hg * 4, hg * 4 + 4)
                hsl_ = slice(hg * 4, hg * 4 + 4)
                PTc = PTb1 if pt_in_1 else NmT
                kPTc = ("PTb1_%d" % hg) if pt_in_1 else K("NmT")
                PTn = NmT if pt_in_1 else PTb1
                kPTn = K("NmT") if pt_in_1 else ("PTb1_%d" % hg)
                kPc = ("PB%d" % hg) if p_in_b else ("PA%d" % hg)
                kPn = ("PA%d" % hg) if p_in_b else ("PB%d" % hg)
                Pc_s = SB if p_in_b else SA
                Pn_s = SA if p_in_b else SB
                kT = "T%d" % hg
                extra = []
                if lev < 5:
                    rsl = slice(128, 384) if p_in_b else slice(0, 256)
                    xo = 0 if p_in_b else 128
                    po = 128 if p_in_b else 0
                    kq1, bank1 = ps()
                    kq2, bank2 = ps()
                    fns = []
                    for h in hs:
                        bk = bank1 if (h - hg * 4) < 2 else bank2
                        o = ((h - hg * 4) % 2) * 256
                        fns.append(MM(bk[:, o:o + 256], PTc[:, h, :], PT3[:, h, rsl]))
                    P.op("pe", fns, reads=[kPTc, kPc, kT] + extra, writes=[kq1, kq2])
                    if DBG_STOP == "L2a":
                        return
                    for bi, (kq_, bk) in enumerate(((kq1, bank1), (kq2, bank2))):
                        bb = bk.rearrange("p (h t) -> p h t", h=2)
                        hh = slice(hg * 4 + bi * 2, hg * 4 + bi * 2 + 2)
                        if L2_DVE:
                            P.op("dve", TT(PT3[:, hh, ST], PT3[:, hh, ST], bb[:, :, xo:xo + 128], ALU.add), reads=[kq_, kT], writes=[kT])
                            P.op("dve", CP(PT3[:, hh, Pn_s], bb[:, :, po:po + 128]), reads=[kq_], writes=[kPn])
                        else:
                            P.op("act", ACTF(PT3[:, hh, Pn_s], bb[:, :, po:po + 128], AF.Copy), reads=[kq_], writes=[kPn])
                            P.op("dve", TT(PT3[:, hh, ST], PT3[:, hh, ST], bb[:, :, xo:xo + 128], ALU.add), reads=[kq_, kT, kPn], writes=[kT])
                else:
                    kq, bank = ps()
                    b3 = v3(bank, 4)
                    P.op("pe", [MM(b3[:, h - hg * 4, :], PTc[:, h, :], PT3[:, h, ST]) for h in hs], reads=[kPTc, kT], writes=[kq])
                    P.op("dve", TT(PT3[:, hsl_, ST], PT3[:, hsl_, ST], b3, ALU.add), reads=[kq, kT], writes=[kT])
                kq, bank = ps()
                b3 = v3(bank, 4)
                P.op("pe", [MM(b3[:, h - hg * 4, :], PT3[:, h, Pc_s], PTc[:, h, :]) for h in hs], reads=[kPTc, kPc] + extra, writes=[kq])
                P.op("act", ACTF(PTn[:, hsl_, :], b3, AF.Copy), reads=[kq] + extra, writes=[kPTn])
                yield
            p_in_b = not p_in_b
            pt_in_1 = not pt_in_1
        for hg in range(2):
            hs = range(hg * 4, hg * 4 + 4)
            hsl_ = slice(hg * 4, hg * 4 + 4)
            PTc = PTb1 if pt_in_1 else NmT
            kPTc = ("PTb1_%d" % hg) if pt_in_1 else K("NmT")
            kT = "T%d" % hg
            kq, bank = ps()
            b3 = v3(bank, 4)
            P.op("pe", [MM(b3[:, h - hg * 4, :], PTc[:, h, :], PT3[:, h, ST]) for h in hs], reads=[kPTc, kT], writes=[kq])
            P.op("dve", TT(PT3[:, hsl_, ST], PT3[:, hsl_, ST], b3, ALU.add), reads=[kq, kT], writes=[kT])
            yield
        if DBG_STOP == "F":
            return
        kq, bank = ps()
        P.op("pe", [MM(bank[:, h * 64:(h + 1) * 64], KAm[:, h, :], hv(Vb, h)) for h in range(8)], reads=[K("KAm"), K("Vb")], writes=[kq])
        P.op("act", ACTF(AZ[:, :, 64:128], v3(bank), AF.Copy), reads=[kq], writes=[K("AZz")])
        yield
        for hg in range(2):
            kq, bank = ps()
            b3 = v3(bank, 4)
            P.op("pe", [MM(b3[:, h - hg * 4, :], TTm[:, h, :], AZ[:, h, :]) for h in range(hg * 4, hg * 4 + 4)], reads=["T%d" % hg, K("AZa"), K("AZz")], writes=[kq])
            P.op("act", ACTF(AV[:, hg * 4:hg * 4 + 4, :], b3, AF.Copy), reads=[kq], writes=["AV"])
            yield
        if own:
            for hg in range(2):
                kq, bank = ps()
                b3 = v3(bank, 4)
                P.op("pe", [MM(b3[0:64, h - hg * 4, :], AZ[:, h, 0:64], TTm[:, h, :]) for h in range(hg * 4, hg * 4 + 4)], reads=["T%d" % hg, K("AZa")], writes=[kq])
                P.op("act", ACTF(AhT[:, hg * 4:hg * 4 + 4, :], b3[0:64], AF.Copy), reads=[kq], writes=["AhT"])
                yield
        for c in range(2):
            pb = 64 * c
            kq, bank = ps()
            P.op("pe", [MM(bank[0:64, h * 64:(h + 1) * 64], AV[pb:pb + 64, h, 0:64], Bt[pb:pb + 64, h * 64:(h + 1) * 64]) for h in range(8)],
                 reads=["AV", K("Bt")], writes=[kq])
            P.op("act", ACTF(PpT[:, c, :, :], v3(bank[0:64, :]), AF.Copy), reads=[kq], writes=["PpT%d" % c])
            kq, bank = ps()
            fns = []
            for h in range(8):
                fns.append(MM(bank[0:64, h * 64:(h + 1) * 64], Bt[pb:pb + 64, h * 64:(h + 1) * 64], AV[pb:pb + 64, h, 64:128], True, False))
                fns.append(MM(bank[0:64, h * 64:(h + 1) * 64], Kt[pb:pb + 64, h * 64:(h + 1) * 64], Vb[pb:pb + 64, h * 64:(h + 1) * 64], False, True))
            P.op("pe", fns, reads=["AV", K("Bt"), K("Kt"), K("Vb")], writes=[kq])
            P.op("act", ACTF(Qp[:, c, :], bank[0:64, :], AF.Copy), reads=[kq], writes=["Qp%d" % c])
            yield
        while i > 0 and not j_done[i - 1]:
            yield
        st_in = []
        for c in range(2):
            nstate = state["n"]
            n16c, n32c = N16[nstate % 4], N32[nstate % 2]
            k16c, k32c = "N16_%d" % (nstate % 4), "N32_%d" % (nstate % 2)
            n16n, n32n = N16[(nstate + 1) % 4], N32[(nstate + 1) % 2]
            k16n, k32n = "N16_%d" % ((nstate + 1) % 4), "N32_%d" % ((nstate + 1) % 2)
            st_in.append((n16c, k16c))
            kq, bank = ps()
            P.op("pe", [MM(bank[0:64, h * 64:(h + 1) * 64], PpT[:, c, h, :], n16c[:, h * 64:(h + 1) * 64]) for h in range(8)],
                 reads=["PpT%d" % c, k16c], writes=[kq])
            P.op("dve", TT(t1, bank[0:64, :], n32c, ALU.add), reads=[kq, k32c], writes=["tmpB"])
            P.op("dve", TT(t1, t1, Qp[:, c, :], ALU.add), reads=["tmpB", "Qp%d" % c], writes=["tmpB"])
            P.op("dve", TT(v3(n32n), v3(t1), Wc[:, :, c:c + 1].to_broadcast([64, 8, 64]), ALU.mult), reads=["tmpB", K("Wc")], writes=[k32n])
            P.op("act", ACTF(n16n, n32n, AF.Copy), reads=[k32n], writes=[k16n])
            state["n"] += 1
        j_done[i] = True
        yield
        if not own:
            return
        kq, bank = ps()
        fns = []
        for c in range(2):
            pb = 64 * c
            for h in range(8):
                fns.append(MM(bank[pb:pb + 64, h * 64:(h + 1) * 64], AhT[:, h, pb:pb + 64], st_in[c][0][:, h * 64:(h + 1) * 64]))
        P.op("pe", fns, reads=["AhT", st_in[0][1], st_in[1][1]], writes=[kq])
        P.op("dve", TT(v3(Usb), v3(bank), AV[:, :, 64:128], ALU.add), reads=[kq, "AV"], writes=["Usb"])
        yield
        kq, bank = ps()
        fns = []
        for h in range(8):
            for c in range(2):
                pb = 64 * c
                fns.append(MM(bank[pb:pb + 64, h * 64:(h + 1) * 64], ART[:, h, 128 + pb:128 + pb + 64], st_in[c][0][:, h * 64:(h + 1) * 64], True, False))
            fns.append(MM(bank[:, h * 64:(h + 1) * 64], BRm[:, h, :], hv(Usb, h), False, False))
            fns.append(MM(bank[:, h * 64:(h + 1) * 64], KRm[:, h, :], hv(Vb, h), False, True))
        P.op("pe", fns, reads=[K("ARTr"), st_in[0][1], st_in[1][1], K("BRm"), K("KRm"), "Usb", K("Vb")], writes=[kq])
        P.op("act", ACTF(y32, bank, AF.Copy), reads=[kq], writes=[Y32])
        yield
        P.op("dve", RED(s8[:, 8:16], v3(y32)), reads=[Y32], writes=["s8b"])
        P.op("dve", TT(tmpB, y32, y32, ALU.mult), reads=[Y32], writes=["tmpB"])
        P.op("dve", RED(s8[:, 16:24], v3(tmpB)), reads=["tmpB"], writes=["s8c"])
        P.op("dve", TS(s8[:, 8:16], s8[:, 8:16], 1.0 / 64), reads=["s8b"], writes=["s8b"])
        P.op("dve", TT(s8[:, 24:32], s8[:, 8:16], s8[:, 8:16], ALU.mult), reads=["s8b"], writes=["s8d"])
        P.op("dve", STT(s8[:, 16:24], s8[:, 16:24], 1.0 / 64, s8[:, 24:32], ALU.mult, ALU.subtract), reads=["s8c", "s8d"], writes=["s8c"])
        P.op("act", ACTF(s8[:, 16:24], s8[:, 16:24], AF.Sqrt, bias=eps_t[:, 1:2]), reads=["s8c", "eps"], writes=["s8c"])
        P.op("dve", lambda e: e.reciprocal(s8[:, 16:24], s8[:, 16:24]), reads=["s8c"], writes=["s8c"])
        yield
        P.op("dve", TT(v3(y32), v3(y32), s8[:, 8:16].unsqueeze(2).to_broadcast([128, 8, 64]), ALU.subtract), reads=[Y32, "s8b"], writes=[Y32])
        P.op("dve", TT(v3(y32), v3(y32), s8[:, 16:24].unsqueeze(2).to_broadcast([128, 8, 64]), ALU.mult), reads=[Y32, "s8c"], writes=[Y32])
        P.op("dve", TT(y32, y32, V1("gng"), ALU.mult), reads=[Y32, "vec1"], writes=[Y32])
        P.op("dve", TT(y32, y32, V1("gnb"), ALU.add), reads=[Y32, "vec1"], writes=[Y32])
        P.op("dve", TT(v3(tmpB), v3(Vb), rkf.unsqueeze(2).to_broadcast([128, 8, 64]), ALU.mult), reads=[K("Vb"), K("rkf")], writes=["tmpB"])
        P.op("dve", TT(y32, y32, tmpB, ALU.add), reads=[Y32, "tmpB"], writes=[Y32])
        P.op("dve", TT(yA, y32, g32, ALU.mult), reads=[Y32, K("g32")], writes=["yA"])
        P.dma("pool", lambda e: e.dma_start(out=ym_scr[(i - OWN_FROM) * 128:(i - OWN_FROM + 1) * 128, 0:512], in_=yA), reads=["yA"], writes=["ymscrA"])
        yield
    def step(g, stream=0):
        ps_stream[0] = stream
        try:
            return next(g), False
        except StopIteration:
            return None, True
        finally:
            ps_stream[0] = 0

    def drive(gen_fn, n_items):
        if not PIPELINE:
            for i in range(n_items):
                for _ in gen_fn(i):
                    pass
            return
        nxt = 1
        a, a_back, b, b_half = gen_fn(0), False, None, False
        while a is not None:
            r, fin = step(a, 2 if a_back else 1)
            if fin:
                a, a_back, b, b_half = b, b_half, None, False
                if a is not None and a_back and nxt < n_items:
                    b = gen_fn(nxt)
                    nxt += 1
                elif a is None and nxt < n_items:
                    a = gen_fn(nxt)
                    nxt += 1
                continue
            if r == "HALF":
                a_back = True
                if b is None and nxt < n_items:
                    b = gen_fn(nxt)
                    nxt += 1
            if b is not None and not b_half:
                r, fin = step(b, 1)
                assert not fin
                if r == "HALF":
                    b_half = True

    drive(tile_gen, n_tiles)

    if debug == "ym":
        return nc, P, es
    P.barrier()
    A.off = phase_mark
    vec2 = A.alloc([128, NV2], F32)

    def V2(nm):
        o, n = O2[nm]
        return vec2[:, o:o + n]

    acc = A.alloc([128, NT_OWN, D], F32)
    wreg = A.alloc([128, 24 * 1024], BF16)
    Wout = wreg[:, 0:8192].rearrange("p (k n) -> p k n", k=8)
    Wpg = wreg[:, 8192:16384].rearrange("p (k n) -> p k n", k=8)
    Wpp = wreg[:, 16384:18432].rearrange("p (k n) -> p k n", k=2)
    wrt = A.alloc([128, 8, 36], BF16)
    sgmb = A.alloc([128, 128], BF16)
    onesb = A.alloc([128, 128], BF16)
    base = A.alloc([128, 32], F32)
    mark2 = A.off
    ymt_ = [A.alloc([128, D], BF16) for _ in range(2)]
    ymT = A.alloc([128, 8, 128], BF16)
    xnt_ = [A.alloc([128, D], F32) for _ in range(2)]
    rowb = [A.alloc([128, ROW], BF16) for _ in range(4)]
    x1Tt_ = [A.alloc([128, 8, 128], BF16) for _ in range(2)]
    ptl_ = [A.alloc([128, 256], F32) for _ in range(2)]
    pbf = A.alloc([128, 256], BF16)
    pT = A.alloc([128, 2, 128], BF16)
    sgt = A.alloc([128, 512], F32)
    lg = A.alloc([128, 36], F32)
    r8 = A.alloc([128, 64], F32)
    selb = A.alloc([128, 32], BF16)
    rsc = A.alloc([128, 32], F32)
    rt32 = A.alloc([128, 4, 32], F32)
    idxi = [A.alloc([128, 2], I32) for _ in range(2)]
    print("phase2a sbuf bytes", A.off)

    P.dma("sp", lambda e: e.dma_start(out=vec2, in_=vec2_d), writes=["vec2"])
    P.dma("sp", lambda e: e.dma_start(out=Wout, in_=wout_b.rearrange("(k p) n -> p k n", p=128)), reads=["woutb"], writes=["Wout"])
    P.dma("sp", lambda e: e.dma_start(out=Wpg, in_=wpg_b.rearrange("(k p) n -> p k n", p=128)), reads=["wpgb"], writes=["Wpg"])
    P.dma("sp", lambda e: e.dma_start(out=Wpp, in_=wpp_b.rearrange("(k p) n -> p k n", p=128)), reads=["wppb"], writes=["Wpp"])
    P.dma("pool", lambda e: e.dma_start(out=wrt, in_=wrt_d.rearrange("(k p) n -> p k n", p=128)), writes=["wrt"])
    P.op("dve", TT(sgmb, m_gm, ident32, ALU.subtract), reads=["cm32"], writes=["sgmb"])
    P.op("pool", lambda e: e.memset(onesb, 1.0), writes=["onesb"])
    P.op("pool", lambda e: e.memset(base, 0.0), writes=["base"])

    def pre_gen(i):
        ymt, xnt, x1Tt, ptl = ymt_[i % 2], xnt_[i % 2], x1Tt_[i % 2], ptl_[i % 2]
        Kp = lambda nm: "%s_%d" % (nm, i % 2)
        P.dma("sp", lambda e: e.dma_start(out=ymt, in_=ym_scr[i * 128:(i + 1) * 128, :]), reads=["ymscrA", "ymscrB"], writes=[Kp("ymt")])
        P.dma("sp", lambda e: e.dma_start(out=xnt, in_=xn_scr[i * 128:(i + 1) * 128, :]), reads=["xnscr"], writes=[Kp("xnt")])
        P.dma("sp", lambda e: e.dma_start(out=ptl, in_=pin[i * 128:(i + 1) * 128, :]), writes=[Kp("ptl")])
        k, pb16 = transposes(ymt, [Kp("ymt")], 8)
        P.op("act", ACTF(ymT, v3(pb16[:, 0:1024]), AF.Copy), reads=[k], writes=["ymT"])
        for hf in range(2):
            kq, bank = ps()
            P.op("pe", [MM(bank, ymT[:, kc, :], Wout[:, kc, hf * 512:(hf + 1) * 512], kc == 0, kc == 7) for kc in range(8)],
                 reads=["ymT", "Wout"], writes=[kq])
            P.op("dve", STT(xnt[:, hf * 512:(hf + 1) * 512], xnt[:, hf * 512:(hf + 1) * 512], ALPHA, bank, ALU.mult, ALU.add),
                 reads=[kq, Kp("xnt")], writes=[Kp("xnt")])
        yield
        ln_tile(xnt, V2("l1g"), V2("l1b"), Kp("xnt"), "vec2")
        yield
        if debug == "x1":
            P.dma("sp", lambda e: e.dma_start(out=dbg_d[i * 128:(i + 1) * 128, :], in_=xnt), reads=[Kp("xnt")], writes=["dbg"])
        ka = ("acc", i)
        r0, r1 = rowb[(2 * i) % 4], rowb[(2 * i + 1) % 4]
        kr0, kr1 = "rowb%d" % ((2 * i) % 4), "rowb%d" % ((2 * i + 1) % 4)
        P.op("act", ACTF(acc[:, i, :], xnt, AF.Copy, scale=ALPHA), reads=[Kp("xnt")], writes=[ka])
        P.op("act", ACTF(r0[:, 0:1024], xnt, AF.Copy), reads=[Kp("xnt")], writes=[kr0])
        P.op("act", ACTF(r1[:, 0:1024], xnt, AF.Copy), reads=[Kp("xnt")], writes=[kr1])
        k, pb16 = transposes(r0[:, 0:1024], [kr0], 8)
        P.op("act", ACTF(x1Tt, v3(pb16[:, 0:1024]), AF.Copy), reads=[k], writes=[Kp("x1Tt")])
        yield
        P.op("act", ACTF(pbf, ptl, AF.Copy), reads=[Kp("ptl")], writes=["pbf"])
        k, pb16 = transposes(pbf, ["pbf"], 2)
        P.op("act", ACTF(pT, v3(pb16[:, 0:256], 2), AF.Copy), reads=[k], writes=["pT"])
        for hf in range(2):
            cs = slice(hf * 512, (hf + 1) * 512)
            kq, bank = ps()
            P.op("pe", [MM(bank, x1Tt[:, kc, :], Wpg[:, kc, cs], kc == 0, kc == 7) for kc in range(8)], reads=[Kp("x1Tt"), "Wpg"], writes=[kq])
            P.op("dve", TT(sgt, bank, V2("bpg")[:, cs], ALU.add), reads=[kq, "vec2"], writes=["sgt"])
            P.op("act", ACTF(sgt, sgt, AF.Sigmoid), reads=["sgt"], writes=["sgt"])
            kq, bank = ps()
            P.op("pe", [MM(bank, pT[:, kc, :], Wpp[:, kc, cs], kc == 0, kc == 1) for kc in range(2)], reads=["pT", "Wpp"], writes=[kq])
            P.op("dve", TT(sgt, sgt, bank, ALU.mult), reads=[kq, "sgt"], writes=["sgt"])
            P.op("dve", TT(acc[:, i, cs], acc[:, i, cs], sgt, ALU.add), reads=["sgt", ka], writes=[ka])
            yield
        yield "HALF"
        kq, bank = ps()
        P.op("pe", [MM(bank[:, 0:36], x1Tt[:, kc, :], wrt[:, kc, :], kc == 0, kc == 7) for kc in range(8)], reads=[Kp("x1Tt"), "wrt"], writes=[kq])
        P.op("dve", TT(lg, bank[:, 0:36], V2("brt"), ALU.add), reads=[kq, "vec2"], writes=["lg"])
        gl = lg[:, 0:4]
        gmax, ngmax, gsum, pg = r8[:, 0:1], r8[:, 1:2], r8[:, 2:3], r8[:, 3:4]
        oh, gex = r8[:, 4:8], r8[:, 8:12]
        el, m1, m2 = r8[:, 12:20], r8[:, 20:21], r8[:, 21:22]
        mk1, mk2, el2 = r8[:, 24:32], r8[:, 32:40], r8[:, 40:48]
        dm, w1, w2 = r8[:, 22:23], r8[:, 23:24], r8[:, 48:49]
        idf = r8[:, 50:52]
        P.op("dve", RED(gmax, gl, ALU.max), reads=["lg"], writes=["r_gmax"])
        P.op("dve", TS(oh, gl, gmax, None, ALU.is_equal), reads=["lg", "r_gmax"], writes=["r_oh"])
        P.op("dve", TS(ngmax, gmax, -1.0), reads=["r_gmax"], writes=["r_ngmax"])
        P.op("act", ACTF(gex, gl, AF.Exp, bias=ngmax), reads=["lg", "r_ngmax"], writes=["r_gex"])
        P.op("dve", RED(gsum, gex), reads=["r_gex"], writes=["r_gsum"])
        P.op("dve", lambda e: e.reciprocal(pg, gsum), reads=["r_gsum"], writes=["r_pg"])
        yield
        P.op("dve", TT(v3(rsc, 4), v3(lg[:, 4:36], 4), oh.unsqueeze(2).to_broadcast([128, 4, 8]), ALU.mult), reads=["lg", "r_oh"], writes=["rsc"])
        P.op("dve", RED(el, rsc.rearrange("p (g e) -> p e g", g=4)), reads=["rsc"], writes=["r_el"])
        P.op("dve", RED(m1, el, ALU.max), reads=["r_el"], writes=["r_m1"])
        P.op("dve", TS(mk1, el, m1, None, ALU.is_equal), reads=["r_el", "r_m1"], writes=["r_mk1"])
        P.op("dve", STT(el2, mk1, -1e30, el, ALU.mult, ALU.add), reads=["r_mk1", "r_el"], writes=["r_el2"])
        P.op("dve", RED(m2, el2, ALU.max), reads=["r_el2"], writes=["r_m2"])
        P.op("dve", TS(mk2, el2, m2, None, ALU.is_equal), reads=["r_el2", "r_m2"], writes=["r_mk2"])
        P.op("dve", TT(dm, m1, m2, ALU.subtract), reads=["r_m1", "r_m2"], writes=["r_dm"])
        P.op("act", ACTF(w1, dm, AF.Sigmoid), reads=["r_dm"], writes=["r_w1"])
        P.op("dve", TS(w2, w1, -1.0, 1.0, ALU.mult, ALU.add), reads=["r_w1"], writes=["r_w2"])
        yield
        P.op("dve", TT(r0[:, 1024:1026].bitcast(F32), w1, pg, ALU.mult), reads=["r_w1", "r_pg", kr0], writes=[kr0])
        P.op("dve", TT(r1[:, 1024:1026].bitcast(F32), w2, pg, ALU.mult), reads=["r_w2", "r_pg", kr1], writes=[kr1])
        P.op("dve", TS(r0[:, 1026:1028].bitcast(I32), V2("pid"), 2.0, float(256 * i), ALU.mult, ALU.add), reads=["vec2", kr0], writes=[kr0])
        P.op("dve", TS(r1[:, 1026:1028].bitcast(I32), V2("pid"), 2.0, float(256 * i + 1), ALU.mult, ALU.add), reads=["vec2", kr1], writes=[kr1])
        s1, s2, sany, posn = rt32[:, 0, :], rt32[:, 1, :], rt32[:, 2, :], rt32[:, 3, :]
        P.op("dve", TT(v3(s1, 4), oh.unsqueeze(2).to_broadcast([128, 4, 8]), mk1.unsqueeze(1).to_broadcast([128, 4, 8]), ALU.mult), reads=["r_oh", "r_mk1"], writes=["s1"])
        P.op("dve", TT(v3(s2, 4), oh.unsqueeze(2).to_broadcast([128, 4, 8]), mk2.unsqueeze(1).to_broadcast([128, 4, 8]), ALU.mult), reads=["r_oh", "r_mk2"], writes=["s2"])
        P.op("dve", TT(selb, s1, s2, ALU.add), reads=["s1", "s2"], writes=["selb"])
        kq, bank = ps()
        P.op("pe", MM(bank[:, 0:32], sgmb, selb), reads=["sgmb", "selb"], writes=[kq])
        P.op("dve", TT(posn, bank[:, 0:32], base, ALU.add), reads=[kq, "base"], writes=["posn"])
        kq, bank = ps()
        P.op("pe", MM(bank[:, 0:32], onesb, selb), reads=["onesb", "selb"], writes=[kq])
        P.op("dve", TT(base, base, bank[:, 0:32], ALU.add), reads=[kq, "base", "posn"], writes=["base"])
        yield
        P.op("dve", TS(sany, posn, float(CAP), 1e6, ALU.is_ge, ALU.mult), reads=["posn"], writes=["sany"])
        P.op("dve", TT(posn, posn, sany, ALU.add), reads=["posn", "sany"], writes=["posn"])
        P.op("dve", TT(posn, posn, V2("eC"), ALU.add), reads=["posn", "vec2"], writes=["posn"])
        P.op("dve", TT(s1, s1, posn, ALU.mult), reads=["s1", "posn"], writes=["s1"])
        P.op("dve", TT(s2, s2, posn, ALU.mult), reads=["s2", "posn"], writes=["s2"])
        P.op("dve", RED(idf, rt32[:, 0:2, :]), reads=["s1", "s2"], writes=["idf"])
        ii = idxi[i % 2]
        kii = "idxi%d" % (i % 2)
        P.op("dve", CP(ii, idf), reads=["idf"], writes=[kii])
        for kk_, (rr, krr) in enumerate(((r0, kr0), (r1, kr1))):
            P.dma("pool", lambda e, rr=rr, ii=ii, kk_=kk_: e.indirect_dma_start(
                out=xg_d[:, :], out_offset=bass.IndirectOffsetOnAxis(ap=ii[:, kk_:kk_ + 1], axis=0), in_=rr[:, :], in_offset=None,
                bounds_check=P.regs["xg"], oob_is_err=False),
                reads=[krr, kii] + ["XgT%d" % t for t in range(NEXP * CAP // 128)], writes=[("Xgs", i, kk_)])

    drive(pre_gen, NT_OWN)

    P.barrier()
    A.off = mark2
    NST = CAP // 128
    xgs = [A.alloc([128, NST, ROW], BF16) for _ in range(2)]
    xgT = [A.alloc([128, 8, CAP], BF16) for _ in range(2)]
    hidb = [A.alloc([128, 4, CAP], BF16) for _ in range(2)]
    hsl = A.alloc([128, CAP], F32)
    yw = [A.alloc([128, D], F32) for _ in range(4)]
    yld = [A.alloc([128, 2, D], F32) for _ in range(2)]
    print("phase2b sbuf bytes", A.off)
    wbufs = []
    for b in range(2):
        o = b * 12288
        wbufs.append((wreg[:, o:o + 4096].rearrange("p (k n) -> p k n", k=8),
                      wreg[:, o + 4096:o + 8192].rearrange("p (k n) -> p k n", k=8),
                      wreg[:, o + 8192:o + 12288].rearrange("p (k n) -> p k n", k=4)))
    ywi = 0
    for e_ in range(NEXP):
        Wg, Wu, Wd = wbufs[e_ % 2]
        kw = "we%d" % (e_ % 2)
        P.dma("sp", lambda e, e_=e_, Wg=Wg: e.dma_start(out=Wg, in_=weg_b[e_].rearrange("(k p) n -> p k n", p=128)), reads=[("wegb", e_)], writes=[kw + "g"])
        P.dma("sp", lambda e, e_=e_, Wu=Wu: e.dma_start(out=Wu, in_=weu_b[e_].rearrange("(k p) n -> p k n", p=128)), reads=[("weub", e_)], writes=[kw + "u"])
        P.dma("sp", lambda e, e_=e_, Wd=Wd: e.dma_start(out=Wd, in_=wed_b[e_].rearrange("(k p) n -> p k n", p=128)), reads=[("wedb", e_)], writes=[kw + "d"])
        xg, kxg = xgs[e_ % 2], "xgs%d" % (e_ % 2)
        xT, kxT = xgT[e_ % 2], "xgT%d" % (e_ % 2)
        hb, khb = hidb[e_ % 2], "hid%d" % (e_ % 2)
        P.dma("sp", lambda e, e_=e_, xg=xg: e.dma_start(out=xg, in_=xg_d[e_ * CAP:(e_ + 1) * CAP, :].rearrange("(t p) r -> p t r", p=128)),
              writes=[kxg])
        for st in range(NST):
            k, pb16 = transposes(xg[:, st, 0:1024], [kxg], 8)
            P.op("act", ACTF(xT[:, :, st * 128:(st + 1) * 128], v3(pb16[:, 0:1024]), AF.Copy), reads=[k], writes=[kxT])
        for fc in range(4):
            fs = slice(fc * 128, (fc + 1) * 128)
            kq, bank = ps()
            fns = [MM(bank[:, 0:CAP], Wg[:, kc, fs], xT[:, kc, :], kc == 0, kc == 7) for kc in range(8)]
            P.op("pe", fns, reads=[kxT, kw + "g"], writes=[kq])
            kq2, bank2 = ps()
            fns = [MM(bank2[:, 0:CAP], Wu[:, kc, fs], xT[:, kc, :], kc == 0, kc == 7) for kc in range(8)]
            P.op("pe", fns, reads=[kxT, kw + "u"], writes=[kq2])
            P.op("act", ACTF(hsl, bank[:, 0:CAP], AF.Silu), reads=[kq], writes=["hsl"])
            P.op("dve", TT(hb[:, fc, :], hsl, bank2[:, 0:CAP], ALU.mult), reads=["hsl", kq2], writes=[khb + "_%d" % fc])
        for st in range(NST):
            y_, ky = yw[ywi % 4], "yw%d" % (ywi % 4)
            ywi += 1
            for dh in range(2):
                cs = slice(dh * 512, (dh + 1) * 512)
                kq, bank = ps()
                P.op("pe", [MM(bank, hb[:, fc, st * 128:(st + 1) * 128], Wd[:, fc, cs], fc == 0, fc == 3) for fc in range(4)],
                     reads=[khb + "_%d" % fc for fc in range(4)] + [kw + "d"], writes=[kq])
                P.op("act", ACTF(y_[:, cs], bank, AF.Copy, scale=xg[:, st, 1024:1026].bitcast(F32)), reads=[kq, kxg], writes=[ky + "_%d" % dh])
            P.dma("pool", lambda e, y_=y_, xg=xg, st=st: e.indirect_dma_start(
                out=yo_d[:, :], out_offset=bass.IndirectOffsetOnAxis(ap=xg[:, st, 1026:1028].bitcast(I32), axis=0), in_=y_[:, :], in_offset=None,
                bounds_check=P.regs["yo"], oob_is_err=False), reads=[ky + "_0", ky + "_1", kxg], writes=[("Yo", e_, st)])
    P.barrier()
    for i in range(NT_OWN):
        yl, kyl = yld[i % 2], "yld%d" % (i % 2)
        P.dma("sp", lambda e, i=i, yl=yl: e.dma_start(out=yl, in_=yo_d[i * 256:(i + 1) * 256, :].rearrange("(p k) n -> p k n", k=2)),
              writes=[kyl])
        P.op("dve", TT(acc[:, i, :], acc[:, i, :], yl[:, 0, :], ALU.add), reads=[kyl, ("acc", i)], writes=[("acc", i)])
        P.op("dve", TT(acc[:, i, :], acc[:, i, :], yl[:, 1, :], ALU.add), reads=[kyl, ("acc", i)], writes=[("acc", i)])
        ln_tile(acc[:, i, :], V2("l2g"), V2("l2b"), ("acc", i), "vec2")
        P.dma("pool", lambda e, i=i: e.dma_start(out=out_d[i * 128:(i + 1) * 128, :], in_=acc[:, i, :]), reads=[("acc", i)], writes=[("out", i)])
    return nc, P, es


def _prep_inputs(inputs):
    f = lambda k: np.asarray(inputs[k], dtype=np.float32)
    x = f("x")
    p = f("p")[0]
    bc = lambda v: np.broadcast_to(np.asarray(v, np.float32).reshape(1, -1), (128, np.asarray(v).size))
    vec1 = np.ascontiguousarray(np.concatenate([
        bc(f("ln_emb_g")), bc(f("ln_emb_b")), bc(f("mu_shift")[0]), bc(f("w0")[0]), bc(f("a0")[0]), bc(f("k_k")[0]),
        bc(f("k_a")[0]), bc(f("r_k")[0].reshape(-1)), bc(f("gn_g")[0]), bc(f("gn_b")[0]), bc(f("gmlp_ln_g")[0]),
        bc(f("gmlp_ln_b")[0])], axis=1))
    brt = np.concatenate([f("b_group_router")[0].reshape(-1), f("b_expert_router")[0].reshape(-1)])
    vec2 = np.ascontiguousarray(np.concatenate([
        bc(f("ln1_g")[0]), bc(f("ln1_b")[0]), bc(f("ln2_g")[0]), bc(f("ln2_b")[0]), bc(f("b_ple_gate")[0]), bc(brt),
        bc(np.arange(NEXP, dtype=np.float32) * CAP), np.arange(128, dtype=np.float32).reshape(128, 1)], axis=1))
    wrouter = np.ascontiguousarray(np.concatenate([f("w_group_router")[0], f("w_expert_router")[0].reshape(D, 32)], axis=1))
    wlora = np.ascontiguousarray(np.concatenate([f("w_decay_up")[0], f("w_iclr_up")[0]], axis=0))
    wsT = np.ascontiguousarray(np.transpose(f("w_spatial")[0], (2, 0, 1)).reshape(128, 512))
    bsp = np.ascontiguousarray(f("b_spatial")[0].T)
    idx = np.arange(128)
    same = (idx[:, None] // 64) == (idx[None, :] // 64)
    ident = np.eye(128, dtype=np.float32)
    m_su = (same & (idx[:, None] < idx[None, :])).astype(np.float32)
    m_iu = (same & (idx[:, None] <= idx[None, :])).astype(np.float32)
    m_sl = (same & (idx[:, None] > idx[None, :])).astype(np.float32)
    tri = m_iu.copy()
    m_gm = (idx[:, None] <= idx[None, :]).astype(np.float32)
    cmats = np.ascontiguousarray(np.concatenate([ident, m_su, m_iu, m_sl, tri, m_gm], axis=1))
    sel = np.zeros((128, 2), np.float32)
    sel[63, 0] = 1.0
    sel[127, 1] = 1.0
    tmpl = np.zeros((128, ROW), dtype=ml_dtypes.bfloat16)
    meta = np.zeros((128, 2), np.int32)
    meta[:, 1] = 1 << 24
    tmpl[:, 1024:1028] = meta.view(ml_dtypes.bfloat16).reshape(128, 4)
    shared = {
        "tmpl": tmpl,
        "w_in": np.ascontiguousarray(f("w_in")[0]), "w_out": np.ascontiguousarray(f("w_out")[0]),
        "wlora": wlora, "wgate": np.ascontiguousarray(f("w_gate_up")[0]), "wrouter": wrouter,
        "w_exp_gate": np.ascontiguousarray(f("w_exp_gate")[0]), "w_exp_up": np.ascontiguousarray(f("w_exp_up")[0]),
        "w_exp_down": np.ascontiguousarray(f("w_exp_down")[0]), "w_ple_gate": np.ascontiguousarray(f("w_ple_gate")[0]),
        "w_ple_proj": np.ascontiguousarray(f("w_ple_proj")[0]), "wsT": wsT, "bsp": bsp, "vec1": vec1, "vec2": vec2,
        "cmats": cmats, "sel": sel,
    }
    in_maps = []
    for c in range(8):
        b, hf = c // 2, c % 2
        xs = np.zeros((SEQ, D), np.float32)
        if hf == 1:
            xs[:] = x[b]
        else:
            xs[HALF:] = x[b, :HALF]
        m = dict(shared)
        m["xs"] = xs
        m["p_s"] = np.ascontiguousarray(p[b, hf * HALF:(hf + 1) * HALF])
        m["flag"] = np.full((128, 1), float(hf), np.float32)
        in_maps.append(m)
    return in_maps


_CACHE = {}


def kernel(**inputs):
    in_maps = _prep_inputs(inputs)
    if "nc" not in _CACHE:
        nc, P, es = build()
        with nc.Block() as block:
            P.replay(block)
        es.close()
        _CACHE["nc"] = nc
    nc = _CACHE["nc"]
    res = run_bass_kernel_spmd(nc, in_maps, core_ids=list(range(8)))
    out = np.zeros((NB, SEQ, D), np.float32)
    for c in range(8):
        b, hf = c // 2, c % 2
        out[b, hf * HALF:(hf + 1) * HALF] = res.results[c]["out"]
    return out
```
